# Optimizing a Trainium2 kernel written in Bass

```python
import math
import jax, jax.numpy as jnp
from jax import lax
import numpy as np

D_MODEL = 2048
BATCH = 4
SEQ = 2048
DEPTH = 1

CHUNK = 64
EPS = 1e-6
MLA_HEADS = 8
QK_NOPE = 128
QK_ROPE = 64
V_HEAD = 128
Q_LORA = 512
KV_LORA = 512
ROPE_THETA = 10000.0
Q_BLOCK = 128
S5_CH = 1024
S5_GROUP = 16
S5_GROUPS = S5_CH // S5_GROUP
S5_STATE = 64
DT_MIN = 1e-3
DT_MAX = 1e-1
N_EXPERTS = 64
TOP_K = 6
N_EXPERT_GROUPS = 8
TOPK_GROUPS = 4
D_EXPERT = 512
D_SHARED = 512
ROUTED_SCALE = 2.5
EXPERT_BLOCK = 128

IN_SPLITS = [Q_LORA, Q_LORA + KV_LORA, Q_LORA + KV_LORA + QK_ROPE,
             Q_LORA + KV_LORA + QK_ROPE + S5_CH, Q_LORA + KV_LORA + QK_ROPE + S5_CH + D_MODEL]
IN_COLS = Q_LORA + KV_LORA + QK_ROPE + S5_CH + 2 * D_MODEL

kernel_name = "hybrid_mla_s5_moe_chunk_causal_block"


def rmsnorm(x, g):
    xf = x.astype(jnp.float32)
    y = xf * lax.rsqrt(jnp.mean(xf * xf, axis=-1, keepdims=True) + EPS)
    return (y * g.astype(jnp.float32)).astype(x.dtype)


def rope(x, cos, sin):
    x1, x2 = jnp.split(x, 2, axis=-1)
    return jnp.concatenate([x1 * cos - x2 * sin, x2 * cos + x1 * sin], axis=-1)


def mla(q_lat, kv_lat, k_pe_raw, positions, g_q, g_kv, w_uq, w_uk, w_uv):
    Bn, L, _ = q_lat.shape
    dt = q_lat.dtype
    q = (rmsnorm(q_lat, g_q) @ w_uq).reshape(Bn, L, MLA_HEADS, QK_NOPE + QK_ROPE)
    q_nope, q_pe = q[..., :QK_NOPE], q[..., QK_NOPE:]
    ckv = rmsnorm(kv_lat, g_kv)
    k_nope = (ckv @ w_uk).reshape(Bn, L, MLA_HEADS, QK_NOPE)
    v = (ckv @ w_uv).reshape(Bn, L, MLA_HEADS, V_HEAD)
    inv_freq = ROPE_THETA ** (-jnp.arange(QK_ROPE // 2, dtype=jnp.float32) / (QK_ROPE // 2))
    ang = positions.astype(jnp.float32)[..., None] * inv_freq
    cos, sin = jnp.cos(ang).astype(dt), jnp.sin(ang).astype(dt)
    q_pe = rope(q_pe, cos[:, :, None, :], sin[:, :, None, :])
    k_pe = rope(k_pe_raw, cos, sin)
    scale = (QK_NOPE + QK_ROPE) ** -0.5
    chunk_id = positions // CHUNK

    def block(i):
        s = i * Q_BLOCK
        qn = lax.dynamic_slice_in_dim(q_nope, s, Q_BLOCK, axis=1)
        qp = lax.dynamic_slice_in_dim(q_pe, s, Q_BLOCK, axis=1)
        qc = lax.dynamic_slice_in_dim(chunk_id, s, Q_BLOCK, axis=1)
        sc = (jnp.einsum('bqhd,bkhd->bhqk', qn, k_nope, preferred_element_type=jnp.float32)
              + jnp.einsum('bqhd,bkd->bhqk', qp, k_pe, preferred_element_type=jnp.float32)) * scale
        mask = chunk_id[:, None, :] <= qc[:, :, None]
        sc = jnp.where(mask[:, None], sc, jnp.float32(-1e30))
        p = jax.nn.softmax(sc, axis=-1).astype(v.dtype)
        return jnp.einsum('bhqk,bkhd->bqhd', p, v)

    out = lax.map(block, jnp.arange(L // Q_BLOCK))
    return jnp.moveaxis(out, 0, 1).reshape(Bn, L, MLA_HEADS * V_HEAD)


def s5(u, a_re, a_im, log_dt, b_re, b_im, c_re, c_im, d_skip, w_glu):
    Bn, L, _ = u.shape
    uf = u.astype(jnp.float32).reshape(Bn, L, S5_GROUPS, S5_GROUP)
    step = jnp.exp(log_dt.astype(jnp.float32))[:, None]
    ar, ai = a_re.astype(jnp.float32), a_im.astype(jnp.float32)
    mag = jnp.exp(ar * step)
    abar_re, abar_im = mag * jnp.cos(ai * step), mag * jnp.sin(ai * step)
    den = ar * ar + ai * ai
    nr, ni = abar_re - 1.0, abar_im
    f_re, f_im = (nr * ar + ni * ai) / den, (ni * ar - nr * ai) / den
    br, bi = b_re.astype(jnp.float32), b_im.astype(jnp.float32)
    bbar_re = f_re[..., None] * br - f_im[..., None] * bi
    bbar_im = f_re[..., None] * bi + f_im[..., None] * br
    bu_re = jnp.einsum('blgc,gpc->blgp', uf, bbar_re)
    bu_im = jnp.einsum('blgc,gpc->blgp', uf, bbar_im)
    a_re_t = jnp.broadcast_to(abar_re, bu_re.shape)
    a_im_t = jnp.broadcast_to(abar_im, bu_im.shape)

    def combine(e1, e2):
        a1r, a1i, b1r, b1i = e1
        a2r, a2i, b2r, b2i = e2
        return (a2r * a1r - a2i * a1i, a2r * a1i + a2i * a1r,
                a2r * b1r - a2i * b1i + b2r, a2r * b1i + a2i * b1r + b2i)

    _, _, xr, xi = lax.associative_scan(combine, (a_re_t, a_im_t, bu_re, bu_im), axis=1)
    y = (jnp.einsum('blgp,gcp->blgc', xr, c_re.astype(jnp.float32))
         - jnp.einsum('blgp,gcp->blgc', xi, c_im.astype(jnp.float32))
         + d_skip.astype(jnp.float32) * uf)
    y = jax.nn.gelu(y.reshape(Bn, L, S5_CH)).astype(u.dtype)
    return y * jax.nn.sigmoid(y @ w_glu)


def route(h, w_router, router_bias):
    N = h.shape[0]
    scores = jax.nn.sigmoid(jnp.einsum('nd,de->ne', h, w_router, preferred_element_type=jnp.float32))
    sel = scores + router_bias.astype(jnp.float32)
    grp = sel.reshape(N, N_EXPERT_GROUPS, N_EXPERTS // N_EXPERT_GROUPS)
    gscore = lax.top_k(grp, 2)[0].sum(-1)
    _, gidx = lax.top_k(gscore, TOPK_GROUPS)
    gmask = jnp.any(gidx[..., None] == jnp.arange(N_EXPERT_GROUPS), axis=-2)
    emask = jnp.repeat(gmask, N_EXPERTS // N_EXPERT_GROUPS, axis=-1)
    _, topi = lax.top_k(jnp.where(emask, sel, -jnp.inf), TOP_K)
    w = jnp.take_along_axis(scores, topi, axis=-1)
    w = w / jnp.sum(w, axis=-1, keepdims=True) * ROUTED_SCALE
    return topi, w.astype(h.dtype)


def routed_experts(xt, topi, topw, w_gate, w_up, w_down):
    N, D = xt.shape
    NK = N * TOP_K
    flat_e = topi.reshape(NK)
    flat_tok = jnp.repeat(jnp.arange(N, dtype=jnp.int32), TOP_K)
    flat_w = topw.reshape(NK)
    order = jnp.argsort(flat_e)
    se, stok, sw = flat_e[order], flat_tok[order], flat_w[order]
    counts = jnp.bincount(flat_e, length=N_EXPERTS)
    padded = (counts + EXPERT_BLOCK - 1) // EXPERT_BLOCK * EXPERT_BLOCK
    start = jnp.cumsum(counts) - counts
    pad_end = jnp.cumsum(padded)
    pad_start = pad_end - padded
    dest = pad_start[se] + jnp.arange(NK) - start[se]
    n_blocks = NK // EXPERT_BLOCK + N_EXPERTS
    P = n_blocks * EXPERT_BLOCK
    buf_tok = jnp.zeros((P,), jnp.int32).at[dest].set(stok)
    buf_w = jnp.zeros((P,), xt.dtype).at[dest].set(sw)
    blk_e = jnp.minimum(jnp.searchsorted(pad_end, jnp.arange(n_blocks) * EXPERT_BLOCK, side='right'),
                        N_EXPERTS - 1)

    def run(args):
        tok, e = args
        xb = xt[tok]
        return (jax.nn.silu(xb @ w_gate[e]) * (xb @ w_up[e])) @ w_down[e]

    y = lax.map(run, (buf_tok.reshape(n_blocks, EXPERT_BLOCK), blk_e)).reshape(P, D)
    return jnp.zeros_like(xt).at[buf_tok].add(y * buf_w[:, None].astype(y.dtype))


def setup_inputs(seed: int = 0) -> dict:
    key = jax.random.key(seed)
    ks = iter(jax.random.split(key, 48))
    f32 = jnp.float32

    def nrm(shape, scale):
        return jax.random.normal(next(ks), shape, f32) * scale

    def gain(shape):
        return 1.0 + nrm(shape, 0.05)

    G, P, Cg, Ld, D = S5_GROUPS, S5_STATE, S5_GROUP, DEPTH, D_MODEL
    x = nrm((BATCH, SEQ, D), 1.0)
    c = nrm((BATCH, D), 1.0)
    positions = (jax.random.randint(next(ks), (BATCH, 1), 0, 64, jnp.int32) * CHUNK
                 + jnp.arange(SEQ, dtype=jnp.int32)[None, :])
    return {
        "x": x, "c": c, "positions": positions,
        "w_ada": nrm((Ld, D, 6 * D), 0.5 * D ** -0.5),
        "b_ada": nrm((Ld, 6 * D), 0.01),
        "g_pre_mix": gain((Ld, D)), "g_post_mix": gain((Ld, D)),
        "g_pre_ffn": gain((Ld, D)), "g_post_ffn": gain((Ld, D)),
        "w_in": nrm((Ld, D, IN_COLS), D ** -0.5),
        "g_q": gain((Ld, Q_LORA)), "g_kv": gain((Ld, KV_LORA)),
        "w_uq": nrm((Ld, Q_LORA, MLA_HEADS * (QK_NOPE + QK_ROPE)), Q_LORA ** -0.5),
        "w_uk": nrm((Ld, KV_LORA, MLA_HEADS * QK_NOPE), KV_LORA ** -0.5),
        "w_uv": nrm((Ld, KV_LORA, MLA_HEADS * V_HEAD), KV_LORA ** -0.5),
        "a_re": -0.5 * jnp.exp(nrm((Ld, G, P), 0.05)),
        "a_im": jnp.pi * jnp.arange(P, dtype=f32) + nrm((Ld, G, P), 0.05),
        "log_dt": jax.random.uniform(next(ks), (Ld, G), f32, math.log(DT_MIN), math.log(DT_MAX)),
        "b_re": nrm((Ld, G, P, Cg), (2 * Cg) ** -0.5),
        "b_im": nrm((Ld, G, P, Cg), (2 * Cg) ** -0.5),
        "c_re": nrm((Ld, G, Cg, P), (2 * P) ** -0.5),
        "c_im": nrm((Ld, G, Cg, P), (2 * P) ** -0.5),
        "d_skip": nrm((Ld, G, Cg), 1.0),
        "w_glu": nrm((Ld, S5_CH, S5_CH), S5_CH ** -0.5),
        "w_br_mla": nrm((Ld, MLA_HEADS * V_HEAD, D), (MLA_HEADS * V_HEAD) ** -0.5),
        "w_br_s5": nrm((Ld, S5_CH, D), S5_CH ** -0.5),
        "w_out": nrm((Ld, D, D), D ** -0.5),
        "w_router": nrm((Ld, D, N_EXPERTS), D ** -0.5),
        "router_bias": nrm((Ld, N_EXPERTS), 0.01),
        "w_exp_gate": nrm((Ld, N_EXPERTS, D, D_EXPERT), D ** -0.5),
        "w_exp_up": nrm((Ld, N_EXPERTS, D, D_EXPERT), D ** -0.5),
        "w_exp_down": nrm((Ld, N_EXPERTS, D_EXPERT, D), D_EXPERT ** -0.5),
        "w_sh_gate": nrm((Ld, D, D_SHARED), D ** -0.5),
        "w_sh_up": nrm((Ld, D, D_SHARED), D ** -0.5),
        "w_sh_down": nrm((Ld, D_SHARED, D), D_SHARED ** -0.5),
    }


def reference(x, c, positions, w_ada, b_ada, g_pre_mix, g_post_mix, g_pre_ffn, g_post_ffn, w_in,
              g_q, g_kv, w_uq, w_uk, w_uv, a_re, a_im, log_dt, b_re, b_im, c_re, c_im, d_skip, w_glu,
              w_br_mla, w_br_s5, w_out, w_router, router_bias, w_exp_gate, w_exp_up, w_exp_down,
              w_sh_gate, w_sh_up, w_sh_down):
    Bn, L, D = x.shape
    for l in range(DEPTH):
        mod = (jax.nn.silu(c) @ w_ada[l] + b_ada[l])[:, None, :]
        sh1, sc1, gt1, sh2, sc2, gt2 = jnp.split(mod, 6, axis=-1)
        h = rmsnorm(x, g_pre_mix[l]) * (1.0 + sc1) + sh1
        q_lat, kv_lat, k_pe, u, gate_mla, gate_s5 = jnp.split(h @ w_in[l], IN_SPLITS, axis=-1)
        y_mla = mla(q_lat, kv_lat, k_pe, positions, g_q[l], g_kv[l], w_uq[l], w_uk[l], w_uv[l]) @ w_br_mla[l]
        y_s5 = s5(u, a_re[l], a_im[l], log_dt[l], b_re[l], b_im[l], c_re[l], c_im[l], d_skip[l],
                  w_glu[l]) @ w_br_s5[l]
        mixed = (jax.nn.sigmoid(gate_mla) * y_mla + jax.nn.sigmoid(gate_s5) * y_s5) @ w_out[l]
        x = x + gt1 * rmsnorm(mixed, g_post_mix[l])
        h = (rmsnorm(x, g_pre_ffn[l]) * (1.0 + sc2) + sh2).reshape(Bn * L, D)
        topi, topw = route(h, w_router[l], router_bias[l])
        shared = (jax.nn.silu(h @ w_sh_gate[l]) * (h @ w_sh_up[l])) @ w_sh_down[l]
        ffn = routed_experts(h, topi, topw, w_exp_gate[l], w_exp_up[l], w_exp_down[l]) + shared
        x = x + gt2 * rmsnorm(ffn.reshape(Bn, L, D), g_post_ffn[l])
    return x
```

```python
import math
from contextlib import ExitStack
import numpy as np
import concourse.bass as bass
import concourse.mybir as mybir
from concourse.bass_utils import run_bass_kernel_spmd

F32 = mybir.dt.float32
BF16 = mybir.dt.bfloat16
I32 = mybir.dt.int32
ALU = mybir.AluOpType
AF = mybir.ActivationFunctionType
AX = mybir.AxisListType

D = 2048
NTOK = 1024
NEXT = 2048
EPS = 1e-6
NE = 64
CAP = 384
TWO_PI = 2.0 * math.pi


class KB:
    def __init__(self, nc, es):
        self.nc = nc
        self.es = es
        self.E = {"pe": nc.tensor, "act": nc.scalar, "dve": nc.vector, "pool": nc.gpsimd, "sp": nc.sync}
        self.esem = {e: es.enter_context(nc.semaphore("s_" + e)) for e in self.E}
        self.ecnt = {e: 0 for e in self.E}
        self.seen = {e: {} for e in self.E}
        self.lastw = {}
        self.readers = {}
        self.dsem = {}
        self.pool = []
        self.bnd_reg = None
        self.nsem = 0
        self.nps = 0
        self.ps_ring = [0, 1, 2, 3, 4, 5]
        self.npb = 0

    def _wait(self, eng, tok):
        key, sem, val = tok
        if eng == "pe" and key == "e:pe":
            return
        if self.seen[eng].get(key, 0) >= val:
            return
        self.E[eng].wait_ge(sem, val)
        self.seen[eng][key] = val

    def _deps(self, eng, r, w):
        toks = []
        for b in r:
            t = self.lastw.get(b)
            if t is not None:
                toks.append(t)
        for b in w:
            t = self.lastw.get(b)
            if t is not None:
                toks.append(t)
            toks.extend(self.readers.get(b, {}).values())
        for t in toks:
            self._wait(eng, t)

    def _update(self, tok, r, w):
        for b in r:
            d = self.readers.setdefault(b, {})
            old = d.get(tok[0])
            if old is None or old[2] < tok[2]:
                d[tok[0]] = tok
        for b in w:
            self.lastw[b] = tok
            self.readers[b] = {}

    def op(self, eng, fn, r=(), w=(), inc=True):
        self._deps(eng, r, w)
        inst = fn(self.E[eng])
        if inc:
            self.ecnt[eng] += 1
            inst.then_inc(self.esem[eng], 1)
            tok = ("e:" + eng, self.esem[eng], self.ecnt[eng])
        else:
            tok = ("e:" + eng, self.esem[eng], self.ecnt[eng] + 1)
        self._update(tok, r, w)
        return inst

    def _ent(self, key):
        if key not in self.dsem:
            if self.pool:
                self.dsem[key] = self.pool.pop()
            else:
                self.nsem += 1
                self.dsem[key] = [self.es.enter_context(self.nc.semaphore("dq%d" % self.nsem)), 0, self.nsem]
        return self.dsem[key]

    def dma(self, q, out, in_, r=(), w=(), key=None, **kw):
        self._deps(q, r, w)
        if key is None:
            key = str(w[0] if w else r[0])
        ent = self._ent(key)
        inst = self.E[q].dma_start(out=out, in_=in_, **kw)
        ent[1] += 16
        inst.then_inc(ent[0], 16)
        tok = ("d:%d" % ent[2], ent[0], ent[1])
        self._update(tok, r, w)
        return inst

    def idma(self, out, out_off, in_, in_off, bound, r=(), w=(), key=None):
        q = "pool"
        self._deps(q, r, w)
        ent = self._ent(key)
        if self.bnd_reg is None:
            self.bnd_reg = self.nc.gpsimd.alloc_register("bnd")
            self.nc.gpsimd.reg_mov(self.bnd_reg, bound)
            self.bnd_val = bound
        assert bound == self.bnd_val
        inst = self.nc.gpsimd.indirect_dma_start(out=out, out_offset=out_off, in_=in_, in_offset=in_off,
                                                 bounds_check=self.bnd_reg, oob_is_err=False)
        ent[1] += 16
        inst.then_inc(ent[0], 16)
        tok = ("d:%d" % ent[2], ent[0], ent[1])
        self._update(tok, r, w)
        return inst

    def barrier(self):
        for e in self.E:
            for e2 in self.E:
                if e2 != e and self.ecnt[e2] > 0:
                    self._wait(e, ("e:" + e2, self.esem[e2], self.ecnt[e2]))
            for key, ent in self.dsem.items():
                if ent[1] > 0:
                    self._wait(e, ("d:%d" % ent[2], ent[0], ent[1]))
        self.pool.extend(self.dsem.values())
        self.dsem = {}
        self.lastw = {}
        self.readers = {}

    def ps(self):
        ring = self.ps_ring
        i = ring[self.nps % len(ring)]
        self.nps += 1
        return self.ps_tiles[i], "ps%d" % i

    def ps_fixed(self, i):
        return self.ps_tiles[i], "ps%d" % i

    def pb(self):
        i = self.npb % len(self.pb_tiles)
        self.npb += 1
        return self.pb_tiles[i], "pb%d" % i


def build_program(dbg=None, stop_after=99, step=99):
    nc = bass.Bass("TRN2", target_bir_lowering=False)
    dbg = dbg or []
    din = {}

    def inp(name, shape, dt=F32):
        din[name] = nc.dram_tensor(name, list(shape), dt, kind="ExternalInput").ap()
        return din[name]

    def scratch(name, shape, dt):
        kind = "ExternalOutput" if name in dbg else "Internal"
        return nc.dram_tensor(name, list(shape), dt, kind=kind).ap()

    x_ctx = inp("x_ctx", [NTOK, D]); x_own = inp("x_own", [NTOK, D])
    pos_row = inp("pos_row", [1, NEXT], I32); pos_col = inp("pos_col", [128, 16], I32)
    c_col = inp("c_col", [128, 16]); flag = inp("flag", [128, 1])
    ident_in = inp("ident", [128, 128]); invf_in = inp("invf", [64, 1]); iota_in = inp("iota128", [128, 128])
    b_ada = inp("b_ada", [1, 6 * D]); gvec_in = inp("gvec", [128, 64])
    gq_in = inp("gq", [128, 4]); gkv_in = inp("gkv", [128, 4])
    w_ada = inp("w_ada", [D, 6 * D]); w_in = inp("w_in", [D, 6208])
    w_uq = inp("w_uq", [512, 1536]); w_uk = inp("w_uk", [512, 1024]); w_uv = inp("w_uv", [512, 1024])
    a_re_sl = inp("a_re_sl", [128, 32]); a_im_sl = inp("a_im_sl", [128, 32]); ldt2 = inp("ldt2", [2, 32])
    b_re_sl = inp("b_re_sl", [128, 32, 16]); b_im_sl = inp("b_im_sl", [128, 32, 16])
    c_re_sl = inp("c_re_sl", [128, 32, 16]); c_im_sl = inp("c_im_sl", [128, 32, 16])
    dcol_in = inp("dcol", [128, 8]); w_glu = inp("w_glu", [1024, 1024])
    w_br_mla = inp("w_br_mla", [1024, D]); w_br_s5 = inp("w_br_s5", [1024, D]); w_out = inp("w_out", [D, D])
    w_router = inp("w_router", [D, NE]); rbias_in = inp("router_bias", [1, NE]); ecap_in = inp("ecap", [1, NE]); ltri_in = inp("ltri", [128, 128])
    w_eg = inp("w_exp_gate", [NE, D, 512]); w_eu = inp("w_exp_up", [NE, D, 512]); w_ed = inp("w_exp_down", [NE, 512, D])
    w_sg = inp("w_sh_gate", [D, 512]); w_su = inp("w_sh_up", [D, 512]); w_sd = inp("w_sh_down", [512, D])
    out_d = nc.dram_tensor("out", [NTOK, D], F32, kind="ExternalOutput").ap()
    s_idx = scratch("s_idx", [128, 64], I32)
    s_x1 = scratch("s_x1", [NTOK, D], F32); s_h2T = scratch("s_h2T", [128, 16, NTOK], BF16)
    s_xe = scratch("s_xe", [NE * CAP, D], BF16); s_ye = scratch("s_ye", [NE * CAP, D], BF16); s_sh = scratch("s_sh", [NTOK, D], F32)
    s_attn = scratch("s_attn", [128, 8, NTOK], BF16)
    s_s5 = scratch("s_s5", [128, 8, NTOK], BF16)

    s_qn = scratch("s_qn", [128, 4, NTOK], BF16)
    s_ckv = scratch("s_ckv", [128, 4, NEXT], BF16)
    s_kpe = scratch("s_kpe", [64, NEXT], BF16)
    s_u = scratch("s_u", [128, 8, NEXT], BF16)
    s_hT = scratch("s_hT", [128, 16, NTOK], BF16)
    s_mod = scratch("s_mod", [128, 96], F32)

    w_ada_v = w_ada.rearrange("(k p) n -> p k n", p=128)
    w_in_v = w_in.rearrange("(k p) n -> p k n", p=128)

    with ExitStack() as es:
        kb = KB(nc, es)
        op, dma = kb.op, kb.dma

        def sb(name, shape, dt=F32, st=None):
            return (st or es).enter_context(nc.sbuf_tensor("t_" + name, list(shape), dt))

        kb.ps_tiles = [es.enter_context(nc.psum_tensor("ps%d" % i, [128, 512], F32)) for i in range(6)]
        kb.pb_tiles = [es.enter_context(nc.psum_tensor("pb%d" % i, [128, 1024], BF16)) for i in range(2)]

        ident_f = sb("ident_f", [128, 128]); ident_b = sb("ident_b", [128, 128], BF16)
        ones_f = sb("ones_f", [128, 128]); ones_b = sb("ones_b", [128, 128], BF16)
        one11 = sb("one11", [1, 1])
        modT = sb("modT", [128, 96])
        vecs = sb("vecs", [128, 6, 16])
        gv = sb("gv", [128, 64])
        flag_t = sb("flag_t", [128, 1])
        sbc = sb("sbc", [128, D]); shbc = sb("shbc", [128, D])
        epsc = sb("epsc", [128, 1])

        dma("sp", ident_f[:], ident_in, w=["ident_f"])
        dma("sp", gv[:], gvec_in, w=["gv"])
        dma("sp", flag_t[:], flag, w=["flag_t"])
        op("dve", lambda e: e.tensor_copy(out=ident_b[:], in_=ident_f[:]), r=["ident_f"], w=["ident_b"])
        op("dve", lambda e: e.memset(ones_f[:], 1.0), w=["ones_f"])
        op("dve", lambda e: e.memset(ones_b[:], 1.0), w=["ones_b"])
        op("dve", lambda e: e.memset(one11[:], 1.0), w=["one11"])
        op("dve", lambda e: e.memset(epsc[:], EPS), w=["epsc"])

        scb = sb("scb", [128, 16], BF16)

        def mod_block(nb, sl, slk, br, brk, rt, rk):
            dma("pool", sl[:], w_ada_v[:, :, nb * 512:(nb + 1) * 512], w=[slk])
            dma("sp", br[:], b_ada[0:1, nb * 512:(nb + 1) * 512], w=[brk])

            def compute():
                pt, pk = kb.ps()
                for k in range(16):
                    op("pe", lambda e, k=k: e.matmul(pt[0:1, :], scb[:, k:k + 1], sl[:, k, :], start=(k == 0), stop=(k == 15)),
                       r=["scb", slk], w=[pk], inc=(k == 15))
                op("dve", lambda e: e.tensor_tensor(out=rt[:], in0=pt[0:1, :], in1=br[:], op=ALU.add), r=[pk, brk], w=[rk])
                pc, pck = kb.ps()
                for j in range(4):
                    op("pe", lambda e, j=j: e.matmul(pc[:, j:j + 1], rt[0:1, j * 128:(j + 1) * 128], one11[0:1, 0:1], start=True, stop=True),
                       r=[rk, "one11"], w=[pck], inc=(j == 3))
                op("dve", lambda e: e.tensor_copy(out=modT[:, nb * 4:(nb + 1) * 4], in_=pc[:, 0:4]), r=[pck], w=["modT"])
            return compute

        def vec_s(dst, sc0, g0):
            op("dve", lambda e: e.scalar_tensor_tensor(out=vecs[:, dst, :], in0=modT[:, sc0:sc0 + 16], scalar=1.0,
                                                       in1=gv[:, g0:g0 + 16], op0=ALU.add, op1=ALU.mult),
               r=["modT", "gv"], w=["vecs"])

        with ExitStack() as p0:
            ccol = sb("ccol", [128, 16], F32, p0)
            ring = [sb("p0ring%d" % i, [128, 16, 512], BF16, p0) for i in range(2)]
            brow = [sb("brow%d" % i, [1, 512], F32, p0) for i in range(2)]
            rowt = [sb("rowt%d" % i, [1, 512], F32, p0) for i in range(2)]
            dma("sp", ccol[:], c_col, w=["ccol"])
            op("act", lambda e: e.activation(out=scb[:], in_=ccol[:], func=AF.Silu), r=["ccol"], w=["scb"])
            for nb in range(8):
                i = nb % 2
                mod_block(nb, ring[i], "p0ring%d" % i, brow[i], "brow%d" % i, rowt[i], "rowt%d" % i)()
            vec_s(0, 16, 0)
            op("dve", lambda e: e.tensor_copy(out=vecs[:, 1, :], in_=modT[:, 0:16]), r=["modT"], w=["vecs"])
            kb.barrier()

        def mod_rest_vectors():
            op("dve", lambda e: e.tensor_tensor(out=vecs[:, 2, :], in0=modT[:, 32:48], in1=gv[:, 16:32], op=ALU.mult), r=["modT", "gv"], w=["vecs"])
            vec_s(3, 64, 32)
            op("dve", lambda e: e.tensor_copy(out=vecs[:, 4, :], in_=modT[:, 48:64]), r=["modT"], w=["vecs"])
            op("dve", lambda e: e.tensor_tensor(out=vecs[:, 5, :], in0=modT[:, 80:96], in1=gv[:, 48:64], op=ALU.mult), r=["modT", "gv"], w=["vecs"])
            if "s_mod" in dbg:
                dma("sp", s_mod, modT[:], r=["modT"], key="dbg")

        bcn = [0]

        def build_bc(dst, dstk, vi, st):
            bcn[0] += 1
            diag = [sb("diag%d_%s_%d" % (i, dstk, bcn[0]), [128, 128], F32, st) for i in range(2)]
            for q4 in range(4):
                pt, pk = kb.ps()
                for j in range(4):
                    fc = q4 * 4 + j
                    dg = diag[fc % 2]; dk = "diag%d_%s_%d" % (fc % 2, dstk, bcn[0])
                    op("dve", lambda e, fc=fc, dg=dg: e.tensor_scalar(out=dg[:], in0=ident_f[:], scalar1=vecs[:, vi, fc:fc + 1], scalar2=None, op0=ALU.mult),
                       r=["ident_f", "vecs"], w=[dk])
                    op("pe", lambda e, j=j, dg=dg: e.matmul(pt[:, j * 128:(j + 1) * 128], ones_f[:], dg[:], start=True, stop=True),
                       r=["ones_f", dk], w=[pk])
                op("act", lambda e, q4=q4: e.copy(out=dst[:, q4 * 512:(q4 + 1) * 512], in_=pt[:]), r=[pk], w=[dstk])

        def prenorm_block(st_tiles, x_src, x_is_dram, tb, hT, hTk, names=("junk", "ss", "rstd", "pn_tmp")):
            xt, xtk, junk, ss, rstd, tmp, htok, htk = st_tiles
            kj, kss, krs, ktm = names
            if x_is_dram:
                dma("sp", xt[:], x_src, w=[xtk])
            op("act", lambda e: e.activation(out=junk[:], in_=xt[:], func=AF.Square, accum_out=ss[:]), r=[xtk], w=[kj, kss])
            op("act", lambda e: e.activation(out=rstd[:], in_=ss[:], func=AF.Sqrt, bias=epsc[:], scale=1.0 / D), r=[kss, "epsc"], w=[krs])
            op("dve", lambda e: e.reciprocal(out=rstd[:], in_=rstd[:]), r=[krs], w=[krs])
            op("dve", lambda e: e.tensor_tensor(out=tmp[:], in0=xt[:], in1=sbc[:], op=ALU.mult), r=[xtk, "sbc"], w=[ktm])
            op("dve", lambda e: e.scalar_tensor_tensor(out=htok[:], in0=tmp[:], scalar=rstd[:, 0:1], in1=shbc[:], op0=ALU.mult, op1=ALU.add),
               r=[ktm, krs, "shbc"], w=[htk])
            for half in range(2):
                pt, pk = kb.pb()
                for j in range(8):
                    fc = half * 8 + j
                    op("pe", lambda e, j=j, fc=fc: e.transpose(pt[:, j * 128:(j + 1) * 128], htok[:, fc * 128:(fc + 1) * 128], ident_b[:]),
                       r=[htk, "ident_b"], w=[pk], inc=(j == 7))
                eng = "act" if half == 0 else "dve"
                if eng == "act":
                    op("act", lambda e: e.copy(out=hT[:, half * 8:(half + 1) * 8, tb * 128:(tb + 1) * 128],
                                               in_=pt[:].rearrange("p (c t) -> p c t", c=8)), r=[pk], w=[hTk])
                else:
                    op("dve", lambda e: e.tensor_copy(out=hT[:, half * 8:(half + 1) * 8, tb * 128:(tb + 1) * 128],
                                                      in_=pt[:].rearrange("p (c t) -> p c t", c=8)), r=[pk], w=[hTk])

        if stop_after >= 1:
            with ExitStack() as p1:
                hT = sb("hT", [128, 16, NTOK], BF16, p1)
                build_bc(sbc, "sbc", 0, p1)
                build_bc(shbc, "shbc", 1, p1)
                xts = [sb("xt%d" % i, [128, D], F32, p1) for i in range(2)]
                junk = sb("junk", [128, D], BF16, p1); ss = sb("ss", [128, 1], F32, p1); rstd = sb("rstd", [128, 1], F32, p1)
                tmp = sb("pn_tmp", [128, D], F32, p1)
                htoks = [sb("htok%d" % i, [128, D], BF16, p1) for i in range(2)]
                ring = [sb("p1ring%d" % i, [128, 16, 512], BF16, p1) for i in range(2)]
                nring = [0]
                wkpe = sb("wkpe", [128, 16, 64], BF16, p1); wrot = sb("wrot", [128, 16, 64], BF16, p1)
                gq = sb("gq", [128, 4], F32, p1); gkv = sb("gkv", [128, 4], F32, p1)
                sqs = [sb("sq%d" % i, [128, 512], BF16, p1) for i in range(4)]
                rbc = sb("rbc", [128, 512], F32, p1)
                lat_o = sb("lat_o", [128, 4, NTOK], BF16, p1); lat_q = sb("lat_q", [128, 4, NTOK], BF16, p1)
                u_o = sb("u_o", [128, 8, NTOK], BF16, p1)
                kpe_o = sb("kpe_o", [64, NTOK], BF16, p1)
                posb = sb("posb", [64, NTOK], I32, p1); ang = sb("ang", [64, NTOK], F32, p1); angi = sb("angi", [64, NTOK], I32, p1)
                cosTs = [sb("cosT%d" % i, [64, NTOK], F32, p1) for i in range(2)]; sinTs = [sb("sinT%d" % i, [64, NTOK], F32, p1) for i in range(2)]
                invf = sb("invf", [64, 1], F32, p1)
                kt1 = sb("kt1", [64, 512], F32, p1); kt2 = sb("kt2", [64, 512], F32, p1)
                dma("sp", gq[:], gq_in, w=["gq"]); dma("sp", gkv[:], gkv_in, w=["gkv"]); dma("sp", invf[:], invf_in, w=["invf"])
                dma("pool", wkpe[:], w_in_v[:, :, 1024:1088], w=["wkpe"])
                op("dve", lambda e: e.tensor_scalar(out=wrot[:, :, 0:32], in0=wkpe[:, :, 32:64], scalar1=-1.0, scalar2=None, op0=ALU.mult), r=["wkpe"], w=["wrot"])
                op("dve", lambda e: e.tensor_copy(out=wrot[:, :, 32:64], in_=wkpe[:, :, 0:32]), r=["wkpe"], w=["wrot"])

                def rope_tables(tok0):
                    pi_ = 0 if tok0 == 0 else 1
                    dma("sp", posb[:], pos_row[0:1, tok0:tok0 + NTOK].partition_broadcast(64), w=["posb"])
                    for (dst, dk, off) in ((sinTs[pi_], "sinT%d" % pi_, 0.0), (cosTs[pi_], "cosT%d" % pi_, 0.25)):
                        op("dve", lambda e, off=off: e.tensor_scalar(out=ang[:], in0=posb[:], scalar1=invf[:, 0:1], scalar2=off, op0=ALU.mult, op1=ALU.add),
                           r=["posb", "invf"], w=["ang"])
                        op("dve", lambda e: e.tensor_copy(out=angi[:], in_=ang[:]), r=["ang"], w=["angi"])
                        op("dve", lambda e: e.tensor_tensor(out=ang[:], in0=ang[:], in1=angi[:], op=ALU.subtract), r=["ang", "angi"], w=["ang"])
                        op("act", lambda e, dst=dst: e.activation(out=dst[:], in_=ang[:], func=AF.Sin, scale=TWO_PI * (1 - 1e-6)), r=["ang"], w=[dk])

                def load_w(c0, ncols=512):
                    i = nring[0] % 2; nring[0] += 1
                    dma("pool", ring[i][:, :, 0:ncols], w_in_v[:, :, c0:c0 + ncols], w=["p1ring%d" % i])
                    return ring[i], "p1ring%d" % i

                def mm_group(pt, pk, wt, wk, c0, m, tt):
                    for k in range(16):
                        op("pe", lambda e, k=k: e.matmul(pt[0:m, :], wt[:, k, c0:c0 + m], hT[:, k, tt * 512:(tt + 1) * 512], start=(k == 0), stop=(k == 15)),
                           r=[wk, "hT%d" % tt], w=[pk], inc=(k == 15))

                def seg_lat(c0, g, gk, tt, lo, lok):
                    wt, wk = load_w(c0)
                    pts = []
                    for c in range(4):
                        pt, pk = kb.ps(); pts.append((pt, pk))
                        mm_group(pt, pk, wt, wk, c * 128, 128, tt)
                        op("act", lambda e, c=c, pt=pt: e.activation(out=sqs[c][:], in_=pt[:], func=AF.Square), r=[pk], w=["sq%d" % c])
                    pss, pssk = kb.ps()
                    for c in range(4):
                        op("pe", lambda e, c=c: e.matmul(pss[:], ones_b[:], sqs[c][:], start=(c == 0), stop=(c == 3)),
                           r=["ones_b", "sq%d" % c], w=[pssk], inc=(c == 3))
                    op("act", lambda e: e.activation(out=rbc[:], in_=pss[:], func=AF.Sqrt, bias=epsc[:], scale=1.0 / 512), r=[pssk, "epsc"], w=["rbc"])
                    op("dve", lambda e: e.reciprocal(out=rbc[:], in_=rbc[:]), r=["rbc"], w=["rbc"])
                    for c in range(4):
                        pt, pk = pts[c]
                        op("dve", lambda e, c=c, pt=pt: e.scalar_tensor_tensor(out=lo[:, c, tt * 512:(tt + 1) * 512], in0=pt[:], scalar=g[:, c:c + 1],
                                                                               in1=rbc[:], op0=ALU.mult, op1=ALU.mult),
                           r=[pk, gk, "rbc"], w=[lok])

                def seg_kpe(tt, pi_):
                    cosT = cosTs[pi_]; sinT = sinTs[pi_]
                    pa, pak = kb.ps(); pbt, pbk = kb.ps()
                    mm_group(pa, pak, wkpe, "wkpe", 0, 64, tt)
                    mm_group(pbt, pbk, wrot, "wrot", 0, 64, tt)
                    sl = slice(tt * 512, (tt + 1) * 512)
                    op("dve", lambda e: e.tensor_tensor(out=kt1[:], in0=pa[0:64, :], in1=cosT[:, sl], op=ALU.mult), r=[pak, "cosT%d" % pi_], w=["kt1"])
                    op("dve", lambda e: e.tensor_tensor(out=kt2[:], in0=pbt[0:64, :], in1=sinT[:, sl], op=ALU.mult), r=[pbk, "sinT%d" % pi_], w=["kt2"])
                    op("dve", lambda e: e.tensor_tensor(out=kpe_o[:, sl], in0=kt1[:], in1=kt2[:], op=ALU.add), r=["kt1", "kt2"], w=["kpe_o"])

                def seg_u(tt, use_flag):
                    for hb in range(2):
                        wt, wk = load_w(1088 + hb * 512)
                        for c in range(4):
                            pt, pk = kb.ps()
                            mm_group(pt, pk, wt, wk, c * 128, 128, tt)
                            if use_flag:
                                op("dve", lambda e, pt=pt, c=c: e.tensor_scalar(out=u_o[:, hb * 4 + c, tt * 512:(tt + 1) * 512], in0=pt[:], scalar1=flag_t[:, 0:1], scalar2=None, op0=ALU.mult),
                                   r=[pk, "flag_t"], w=["u_o"])
                            else:
                                op("act", lambda e, pt=pt, c=c: e.copy(out=u_o[:, hb * 4 + c, tt * 512:(tt + 1) * 512], in_=pt[:]), r=[pk], w=["u_o"])

                passes = ((x_ctx, 0), (x_own, NTOK))

                def PN(ps_i, tt):
                    xsrc, tok0 = passes[ps_i]
                    if tt == 0:
                        rope_tables(tok0)
                    for tb in range(tt * 4, tt * 4 + 4):
                        i = tb % 2
                        prenorm_block((xts[i], "xt%d" % i, junk, ss, rstd, tmp, htoks[i], "htok%d" % i), xsrc[tb * 128:(tb + 1) * 128, :], True, tb, hT, "hT%d" % tt)

                def MM(ps_i, tt):
                    xsrc, tok0 = passes[ps_i]
                    if ps_i == 1:
                        seg_lat(0, gq, "gq", tt, lat_q, "lat_q")
                        if tt == 1:
                            dma("sp", s_qn, lat_q[:], r=["lat_q"], key="st_latq")
                    seg_lat(512, gkv, "gkv", tt, lat_o, "lat_o")
                    if tt == 1:
                        dma("sp", s_ckv[:, :, tok0:tok0 + NTOK], lat_o[:], r=["lat_o"], key="st_lat")
                    seg_kpe(tt, ps_i)
                    seg_u(tt, ps_i == 0)
                    if tt == 1:
                        dma("sp", s_kpe[:, tok0:tok0 + NTOK], kpe_o[:], r=["kpe_o"], key="st_kpe")
                        dma("sp", s_u[:, :, tok0:tok0 + NTOK], u_o[:], r=["u_o"], key="st_u")

                PN(0, 0); PN(0, 1); MM(0, 0); PN(1, 0); MM(0, 1); PN(1, 1); MM(1, 0); MM(1, 1)
                dma("sp", s_hT, hT[:], r=["hT0", "hT1"], key="st_hT")
                kb.barrier()


        if stop_after >= 2:
            with ExitStack() as p2:
                qn = sb("qn", [128, 4, NTOK], BF16, p2); ckv = sb("ckv", [128, 4, NEXT], BF16, p2); kpe = sb("kpe", [64, NEXT], BF16, p2)
                wuq = sb("wuq", [128, 4, 1536], BF16, p2); wuk = sb("wuk", [128, 4, 1024], BF16, p2); wuv = sb("wuv", [128, 4, 1024], BF16, p2)
                wqr = sb("wqr", [128, 4, 8, 64], BF16, p2)
                dma("sp", qn[:], s_qn, w=["qn"]); dma("sp", ckv[:], s_ckv, w=["ckv"]); dma("sp", kpe[:], s_kpe, w=["kpe"])
                dma("pool", wuq[:], w_uq.rearrange("(k p) n -> p k n", p=128), w=["wuq"])
                dma("pool", wuk[:], w_uk.rearrange("(k p) n -> p k n", p=128), w=["wuk"])
                dma("pool", wuv[:], w_uv.rearrange("(k p) n -> p k n", p=128), w=["wuv"])
                wuq4 = wuq[:].rearrange("p k (h d) -> p k h d", h=8)
                for k in range(4):
                    op("dve", lambda e, k=k: e.tensor_scalar(out=wqr[:, k, :, 0:32], in0=wuq4[:, k, :, 160:192], scalar1=-1.0, scalar2=None, op0=ALU.mult), r=["wuq"], w=["wqr"])
                    op("dve", lambda e, k=k: e.tensor_copy(out=wqr[:, k, :, 32:64], in_=wuq4[:, k, :, 128:160]), r=["wuq"], w=["wqr"])
                posb = sb("posb2", [64, NTOK], I32, p2); ang = sb("ang2", [64, NTOK], F32, p2); angi = sb("angi2", [64, NTOK], I32, p2)
                cosT = sb("cosT2", [64, NTOK], F32, p2); sinT = sb("sinT2", [64, NTOK], F32, p2); invf = sb("invf2", [64, 1], F32, p2)
                dma("sp", invf[:], invf_in, w=["invf2"])
                dma("sp", posb[:], pos_row[0:1, NTOK:NEXT].partition_broadcast(64), w=["posb2"])
                for (dst, dk, off) in ((sinT, "sinT2", 0.0), (cosT, "cosT2", 0.25)):
                    op("dve", lambda e, off=off: e.tensor_scalar(out=ang[:], in0=posb[:], scalar1=invf[:, 0:1], scalar2=off, op0=ALU.mult, op1=ALU.add), r=["posb2", "invf2"], w=["ang2"])
                    op("dve", lambda e: e.tensor_copy(out=angi[:], in_=ang[:]), r=["ang2"], w=["angi2"])
                    op("dve", lambda e: e.tensor_tensor(out=ang[:], in0=ang[:], in1=angi[:], op=ALU.subtract), r=["ang2", "angi2"], w=["ang2"])
                    op("act", lambda e, dst=dst: e.activation(out=dst[:], in_=ang[:], func=AF.Sin, scale=TWO_PI * (1 - 1e-6)), r=["ang2"], w=[dk])
                pqi = sb("pqi", [128, NTOK], I32, p2); cq = sb("cq", [128, NTOK], F32, p2)
                pki = sb("pki", [128, 16], I32, p2); ck = sb("ck", [128, 16], F32, p2)
                cb = sb("cb", [128, 8], F32, p2); msk = sb("msk", [128, 8, 128], BF16, p2)
                dma("sp", pqi[:], pos_row[0:1, NTOK:NEXT].partition_broadcast(128), w=["pqi"])
                dma("sp", pki[:], pos_col, w=["pki"])
                op("dve", lambda e: e.tensor_single_scalar(out=pqi[:], in_=pqi[:], scalar=6, op=ALU.arith_shift_right), r=["pqi"], w=["pqi"])
                op("dve", lambda e: e.tensor_copy(out=cq[:], in_=pqi[:]), r=["pqi"], w=["cq"])
                op("dve", lambda e: e.tensor_single_scalar(out=pki[:], in_=pki[:], scalar=6, op=ALU.arith_shift_right), r=["pki"], w=["pki"])
                op("dve", lambda e: e.tensor_copy(out=ck[:], in_=pki[:]), r=["pki"], w=["ck"])
                op("dve", lambda e: e.tensor_scalar(out=cb[:], in0=ck[:, 0:8], scalar1=cq[:, 0:1], scalar2=-30000.0, op0=ALU.is_gt, op1=ALU.mult), r=["ck", "cq"], w=["cb"])
                for j in range(8):
                    op("dve", lambda e, j=j: e.tensor_scalar(out=msk[:, j, :], in0=cq[:, j * 128:(j + 1) * 128], scalar1=ck[:, 8 + j:9 + j], scalar2=None, op0=ALU.is_ge), r=["cq", "ck"], w=["msk"])
                attnT = sb("attnT", [128, 8, NTOK], BF16, p2)
                qhn = [sb("qhn%d" % i, [128, NTOK], BF16, p2) for i in range(2)]
                qhp = [sb("qhp%d" % i, [64, NTOK], BF16, p2) for i in range(2)]
                khs = [sb("kh%d" % i, [128, NEXT], BF16, p2) for i in range(2)]
                vhs = [sb("vh%d" % i, [128, 16, 128], BF16, p2) for i in range(2)]
                pTs = [sb("pT%d" % i, [128, 512], BF16, p2) for i in range(4)]
                qt1 = sb("qt1", [64, 512], F32, p2); qt2 = sb("qt2", [64, 512], F32, p2)
                rden = sb("rden", [128, 512], F32, p2)
                npT = [0]
                SC = float(192 ** -0.5)
                mring = [sb("p2ring%d" % i, [128, 16, 512], BF16, p2) for i in range(2)]
                mbrow = [sb("mbrow%d" % i, [1, 512], F32, p2) for i in range(2)]
                mrowt = [sb("mrowt%d" % i, [1, 512], F32, p2) for i in range(2)]
                for h in range(8):
                    b = h % 2
                    mod_pending = [mod_block(8 + 2 * h + i, mring[i], "p2ring%d" % i, mbrow[i], "mbrow%d" % i, mrowt[i], "mrowt%d" % i) for i in range(2)]
                    qn_h, qp_h, kh, vh = qhn[b], qhp[b], khs[b], vhs[b]
                    kq, kp, kk, kv = "qhn%d" % b, "qhp%d" % b, "kh%d" % b, "vh%d" % b
                    for tt in range(2):
                        sl = slice(tt * 512, (tt + 1) * 512)
                        pt, pk = kb.ps()
                        for k in range(4):
                            op("pe", lambda e, k=k: e.matmul(pt[:], wuq[:, k, 192 * h:192 * h + 128], qn[:, k, sl], start=(k == 0), stop=(k == 3)), r=["wuq", "qn"], w=[pk], inc=(k == 3))
                        op("act", lambda e: e.copy(out=qn_h[:, sl], in_=pt[:]), r=[pk], w=[kq])
                        pa, pak = kb.ps(); pbt, pbk = kb.ps()
                        for k in range(4):
                            op("pe", lambda e, k=k: e.matmul(pa[0:64, :], wuq[:, k, 192 * h + 128:192 * h + 192], qn[:, k, sl], start=(k == 0), stop=(k == 3)), r=["wuq", "qn"], w=[pak], inc=(k == 3))
                        for k in range(4):
                            op("pe", lambda e, k=k: e.matmul(pbt[0:64, :], wqr[:, k, h, :], qn[:, k, sl], start=(k == 0), stop=(k == 3)), r=["wqr", "qn"], w=[pbk], inc=(k == 3))
                        op("dve", lambda e: e.tensor_tensor(out=qt1[:], in0=pa[0:64, :], in1=cosT[:, sl], op=ALU.mult), r=[pak, "cosT2"], w=["qt1"])
                        op("dve", lambda e: e.tensor_tensor(out=qt2[:], in0=pbt[0:64, :], in1=sinT[:, sl], op=ALU.mult), r=[pbk, "sinT2"], w=["qt2"])
                        op("dve", lambda e: e.tensor_tensor(out=qp_h[:, sl], in0=qt1[:], in1=qt2[:], op=ALU.add), r=["qt1", "qt2"], w=[kp])
                    for t4 in range(4):
                        sl = slice(t4 * 512, (t4 + 1) * 512)
                        pt, pk = kb.ps()
                        for k in range(4):
                            op("pe", lambda e, k=k: e.matmul(pt[:], wuk[:, k, 128 * h:128 * h + 128], ckv[:, k, sl], start=(k == 0), stop=(k == 3)), r=["wuk", "ckv"], w=[pk], inc=(k == 3))
                        op("act", lambda e: e.copy(out=kh[:, sl], in_=pt[:]), r=[pk], w=[kk])
                    for t4 in range(4):
                        pt, pk = kb.ps()
                        for j in range(4):
                            tb = t4 * 4 + j
                            for k in range(4):
                                op("pe", lambda e, k=k, tb=tb, j=j: e.matmul(pt[:, j * 128:(j + 1) * 128], ckv[:, k, tb * 128:(tb + 1) * 128], wuv[:, k, 128 * h:128 * h + 128],
                                                                        start=(k == 0), stop=(k == 3)), r=["wuv", "ckv"], w=[pk], inc=(k == 3 and j == 3))
                        op("dve", lambda e: e.tensor_copy(out=vh[:, t4 * 4:(t4 + 1) * 4, :], in_=pt[:].rearrange("p (j d) -> p j d", j=4)), r=[pk], w=[kv])
                    kb.ps_ring = [0, 1, 2, 3]
                    for qt in range(2):
                        po, pok = kb.ps_fixed(4); pd, pdk = kb.ps_fixed(5)
                        blocks = [(jb, 0, True) for jb in range(8)] + [(8 + j, max(0, j - 4 * qt) * 128, False) for j in range(4 * qt + 4)]
                        def emit_S(bi):
                            eb, c0, is_ctx = blocks[bi]
                            pss, pssk = kb.ps()
                            q0 = qt * 512 + c0
                            op("pe", lambda e: e.matmul(pss[:, c0:512], kh[:, eb * 128:(eb + 1) * 128], qn_h[:, q0:qt * 512 + 512], start=True, stop=False), r=[kk, kq], w=[pssk], inc=False)
                            op("pe", lambda e: e.matmul(pss[:, c0:512], kpe[:, eb * 128:(eb + 1) * 128], qp_h[:, q0:qt * 512 + 512], start=False, stop=True), r=["kpe", kp], w=[pssk])
                            pi = npT[0] % 4; npT[0] += 1
                            pT = pTs[pi]; pTk = "pT%d" % pi
                            if is_ctx:
                                op("act", lambda e: e.activation(out=pT[:, c0:512], in_=pss[:, c0:512], func=AF.Exp, bias=cb[:, eb:eb + 1], scale=SC), r=[pssk, "cb"], w=[pTk])
                            else:
                                op("act", lambda e: e.activation(out=pT[:, c0:512], in_=pss[:, c0:512], func=AF.Exp, scale=SC), r=[pssk], w=[pTk])
                                j = eb - 8
                                if j >= 4 * qt:
                                    op("pool", lambda e: e.tensor_tensor(out=pT[:, c0:c0 + 128], in0=pT[:, c0:c0 + 128], in1=msk[:, j, :], op=ALU.mult), r=[pTk, "msk"], w=[pTk])
                            return pT, pTk

                        def emit_PV(bi, pT, pTk):
                            eb, c0, is_ctx = blocks[bi]
                            last = (bi == len(blocks) - 1)
                            op("pe", lambda e: e.matmul(po[:, c0:512], vh[:, eb, :], pT[:, c0:512], start=(bi == 0), stop=last), r=[kv, pTk], w=[pok], inc=False)
                            op("pe", lambda e: e.matmul(pd[:, c0:512], ones_b[:], pT[:, c0:512], start=(bi == 0), stop=last), r=["ones_b", pTk], w=[pdk])

                        pend = [emit_S(0), emit_S(1)]
                        for bi in range(len(blocks)):
                            if bi + 2 < len(blocks):
                                pend.append(emit_S(bi + 2))
                            pT, pTk = pend.pop(0)
                            emit_PV(bi, pT, pTk)
                        op("dve", lambda e: e.reciprocal(out=rden[:], in_=pd[:]), r=[pdk], w=["rden"])
                        op("dve", lambda e: e.tensor_tensor(out=attnT[:, h, qt * 512:(qt + 1) * 512], in0=po[:], in1=rden[:], op=ALU.mult), r=[pok, pdk, "rden"], w=["attnT"])
                    kb.ps_ring = [0, 1, 2, 3, 4, 5]
                    for f_ in mod_pending:
                        f_()
                mod_rest_vectors()
                dma("sp", s_attn, attnT[:], r=["attnT"], key="st_attn")
                kb.barrier()


        if stop_after >= 3:
            with ExitStack() as p3:
                yT = sb("yT", [128, 8, NTOK], BF16, p3)
                with ExitStack() as p3a:
                    def small(name):
                        return sb(name, [128, 32], F32, p3a)
                    are = small("are"); aim = small("aim"); ldt = small("ldt"); stp = small("stp"); mag = small("mag"); th = small("th")
                    th2 = small("th2"); fr = small("fr"); fri = sb("fri", [128, 32], I32, p3a); cs = small("cs"); sn = small("sn")
                    abr = small("abr"); abi = small("abi"); den = small("den"); t_a = small("t_a"); t_b = small("t_b")
                    fre = small("fre"); fim = small("fim"); nfim = small("nfim"); e_c = small("e_c"); e_s = small("e_s")
                    ini_r = small("ini_r"); ini_i = small("ini_i"); c_r = small("c_r"); c_i = small("c_i"); l_a = small("l_a"); l_b = small("l_b")
                    bsr = sb("bsr", [128, 32, 16], F32, p3a); bsi = sb("bsi", [128, 32, 16], F32, p3a)
                    csr = sb("csr", [128, 32, 16], F32, p3a); csi = sb("csi", [128, 32, 16], F32, p3a)
                    bbr = sb("bbr", [128, 32, 16], F32, p3a); bbi = sb("bbi", [128, 32, 16], F32, p3a); tb16 = sb("tb16", [128, 16], F32, p3a)
                    dcol = sb("dcol", [128, 8], F32, p3a); iot = sb("iot", [128, 128], F32, p3a); m0 = sb("m0", [128, 128], F32, p3a)
                    dma("sp", are[:], a_re_sl, w=["are"]); dma("sp", aim[:], a_im_sl, w=["aim"])
                    dma("sp", ldt[0:64, :], ldt2[0:1, :].partition_broadcast(64), w=["ldt"]); dma("sp", ldt[64:128, :], ldt2[1:2, :].partition_broadcast(64), w=["ldt"], key="ldt_b")
                    dma("sp", bsr[:], b_re_sl, w=["bsr"]); dma("sp", bsi[:], b_im_sl, w=["bsi"]); dma("sp", csr[:], c_re_sl, w=["csr"]); dma("sp", csi[:], c_im_sl, w=["csi"])
                    dma("sp", dcol[:], dcol_in, w=["dcol"]); dma("sp", iot[:], iota_in, w=["iot"])
                    V = lambda f, r, w: op("dve", f, r=r, w=w)
                    A = lambda f, r, w: op("act", f, r=r, w=w)

                    def sincos(src, srck, off, dst, dstk, tf, tfk, ti, tik):
                        V(lambda e: e.tensor_scalar(out=tf, in0=src, scalar1=off, scalar2=None, op0=ALU.add), [srck], [tfk])
                        V(lambda e: e.tensor_copy(out=ti, in_=tf), [tfk], [tik])
                        V(lambda e: e.tensor_tensor(out=tf, in0=tf, in1=ti, op=ALU.subtract), [tfk, tik], [tfk])
                        A(lambda e: e.activation(out=dst, in_=tf, func=AF.Sin, scale=TWO_PI * (1 - 1e-6)), [tfk], [dstk])

                    A(lambda e: e.activation(out=stp[:], in_=ldt[:], func=AF.Exp), ["ldt"], ["stp"])
                    V(lambda e: e.tensor_tensor(out=t_a[:], in0=are[:], in1=stp[:], op=ALU.mult), ["are", "stp"], ["t_a"])
                    A(lambda e: e.activation(out=mag[:], in_=t_a[:], func=AF.Exp), ["t_a"], ["mag"])
                    V(lambda e: e.tensor_tensor(out=th[:], in0=aim[:], in1=stp[:], op=ALU.mult), ["aim", "stp"], ["th"])
                    V(lambda e: e.tensor_scalar(out=th2[:], in0=th[:], scalar1=1.0 / TWO_PI, scalar2=None, op0=ALU.mult), ["th"], ["th2"])
                    sincos(th2[:], "th2", 0.0, sn[:], "sn", fr[:], "fr", fri[:], "fri")
                    sincos(th2[:], "th2", 0.25, cs[:], "cs", fr[:], "fr", fri[:], "fri")
                    V(lambda e: e.tensor_tensor(out=abr[:], in0=mag[:], in1=cs[:], op=ALU.mult), ["mag", "cs"], ["abr"])
                    V(lambda e: e.tensor_tensor(out=abi[:], in0=mag[:], in1=sn[:], op=ALU.mult), ["mag", "sn"], ["abi"])
                    V(lambda e: e.tensor_tensor(out=den[:], in0=are[:], in1=are[:], op=ALU.mult), ["are"], ["den"])
                    V(lambda e: e.tensor_tensor(out=t_a[:], in0=aim[:], in1=aim[:], op=ALU.mult), ["aim"], ["t_a"])
                    V(lambda e: e.tensor_tensor(out=den[:], in0=den[:], in1=t_a[:], op=ALU.add), ["den", "t_a"], ["den"])
                    V(lambda e: e.reciprocal(out=den[:], in_=den[:]), ["den"], ["den"])
                    V(lambda e: e.tensor_scalar(out=abr[:], in0=abr[:], scalar1=-1.0, scalar2=None, op0=ALU.add), ["abr"], ["abr"])
                    V(lambda e: e.tensor_tensor(out=t_a[:], in0=abr[:], in1=are[:], op=ALU.mult), ["abr", "are"], ["t_a"])
                    V(lambda e: e.tensor_tensor(out=t_b[:], in0=abi[:], in1=aim[:], op=ALU.mult), ["abi", "aim"], ["t_b"])
                    V(lambda e: e.tensor_tensor(out=t_a[:], in0=t_a[:], in1=t_b[:], op=ALU.add), ["t_a", "t_b"], ["t_a"])
                    V(lambda e: e.tensor_tensor(out=fre[:], in0=t_a[:], in1=den[:], op=ALU.mult), ["t_a", "den"], ["fre"])
                    V(lambda e: e.tensor_tensor(out=t_a[:], in0=abi[:], in1=are[:], op=ALU.mult), ["abi", "are"], ["t_a"])
                    V(lambda e: e.tensor_tensor(out=t_b[:], in0=abr[:], in1=aim[:], op=ALU.mult), ["abr", "aim"], ["t_b"])
                    V(lambda e: e.tensor_tensor(out=t_a[:], in0=t_a[:], in1=t_b[:], op=ALU.subtract), ["t_a", "t_b"], ["t_a"])
                    V(lambda e: e.tensor_tensor(out=fim[:], in0=t_a[:], in1=den[:], op=ALU.mult), ["t_a", "den"], ["fim"])
                    V(lambda e: e.tensor_scalar(out=nfim[:], in0=fim[:], scalar1=-1.0, scalar2=None, op0=ALU.mult), ["fim"], ["nfim"])
                    V(lambda e: e.tensor_scalar(out=t_b[:], in0=th2[:], scalar1=128.0, scalar2=None, op0=ALU.mult), ["th2"], ["t_b"])
                    sincos(t_b[:], "t_b", 0.0, e_s[:], "e_s", fr[:], "fr", fri[:], "fri")
                    sincos(t_b[:], "t_b", 0.25, e_c[:], "e_c", fr[:], "fr", fri[:], "fri")
                    for i in range(32):
                        V(lambda e, i=i: e.tensor_scalar(out=tb16[:], in0=bsr[:, i, :], scalar1=fre[:, i:i + 1], scalar2=None, op0=ALU.mult), ["bsr", "fre"], ["tb16"])
                        V(lambda e, i=i: e.scalar_tensor_tensor(out=bbr[:, i, :], in0=bsi[:, i, :], scalar=nfim[:, i:i + 1], in1=tb16[:], op0=ALU.mult, op1=ALU.add), ["bsi", "nfim", "tb16"], ["bbr"])
                        V(lambda e, i=i: e.tensor_scalar(out=tb16[:], in0=bsi[:, i, :], scalar1=fre[:, i:i + 1], scalar2=None, op0=ALU.mult), ["bsi", "fre"], ["tb16"])
                        V(lambda e, i=i: e.scalar_tensor_tensor(out=bbi[:, i, :], in0=bsr[:, i, :], scalar=fim[:, i:i + 1], in1=tb16[:], op0=ALU.mult, op1=ALU.add), ["bsr", "fim", "tb16"], ["bbi"])
                    W1 = sb("W1", [128, 32, 128], BF16, p3a); W2 = sb("W2", [128, 32, 128], BF16, p3a)
                    LBr = sb("LBr", [128, 32, 128], BF16, p3a); LBi = sb("LBi", [128, 32, 128], BF16, p3a)
                    Dg = sb("Dg", [128, 8, 128], BF16, p3a)

                    def place(Wt, Wk, src, srck, neg=False):
                        V(lambda e: e.memset(Wt[:], 0.0), [], [Wk])
                        W4 = Wt[:].rearrange("p (j r) c -> p j r c", r=4)
                        S4 = src[:].rearrange("p (j r) c -> p j r c", r=4)
                        for r in range(4):
                            for gp in range(2):
                                ps_ = slice(64 * gp, 64 * gp + 64)
                                c0 = (2 * r + gp) * 16
                                if neg:
                                    V(lambda e, r=r, ps_=ps_, c0=c0: e.tensor_scalar(out=W4[ps_, :, r, c0:c0 + 16], in0=S4[ps_, :, r, :], scalar1=-1.0, scalar2=None, op0=ALU.mult), [srck], [Wk])
                                else:
                                    V(lambda e, r=r, ps_=ps_, c0=c0: e.tensor_copy(out=W4[ps_, :, r, c0:c0 + 16], in_=S4[ps_, :, r, :]), [srck], [Wk])

                    place(W1, "W1", bbr, "bbr"); place(W2, "W2", bbi, "bbi")
                    for (Wt, Wk, Lt, Lk) in ((W1, "W1", LBr, "LBr"), (W2, "W2", LBi, "LBi")):
                        for i8 in range(4):
                            pt, pk = kb.pb()
                            for q in range(8):
                                i = i8 * 8 + q
                                op("pe", lambda e, i=i, q=q: e.transpose(pt[:, q * 128:(q + 1) * 128], Wt[:, i, :], ident_b[:]), r=[Wk, "ident_b"], w=[pk], inc=(q == 7))
                            V(lambda e, i8=i8: e.tensor_copy(out=Lt[:, i8 * 8:(i8 + 1) * 8, :], in_=pt[:].rearrange("p (q c) -> p q c", q=8)), [pk], [Lk])
                    place(W1, "W1", csr, "csr"); place(W2, "W2", csi, "csi", neg=True)
                    for j in range(8):
                        V(lambda e, j=j: e.tensor_scalar(out=Dg[:, j, :], in0=ident_f[:], scalar1=dcol[:, j:j + 1], scalar2=None, op0=ALU.mult), ["ident_f", "dcol"], ["Dg"])
                    cosT = sb("s5cos", [128, 32, 128], BF16, p3a); sinT = sb("s5sin", [128, 32, 128], BF16, p3a); dtab = sb("dtab", [128, 32, 128], F32, p3a)
                    tf = sb("tf", [128, 128], F32, p3a); tfi = sb("tfi", [128, 128], I32, p3a)
                    V(lambda e: e.tensor_scalar(out=m0[:], in0=iot[:], scalar1=0.5, scalar2=None, op0=ALU.is_gt), ["iot"], ["m0"])
                    for i in range(32):
                        for (dst, dk, off) in ((sinT, "s5sin", 0.0), (cosT, "s5cos", 0.25)):
                            V(lambda e, i=i, off=off: e.tensor_scalar(out=tf[:], in0=iot[:], scalar1=th2[:, i:i + 1], scalar2=off, op0=ALU.mult, op1=ALU.add), ["iot", "th2"], ["tf"])
                            V(lambda e: e.tensor_copy(out=tfi[:], in_=tf[:]), ["tf"], ["tfi"])
                            V(lambda e: e.tensor_tensor(out=tf[:], in0=tf[:], in1=tfi[:], op=ALU.subtract), ["tf", "tfi"], ["tf"])
                            A(lambda e, i=i, dst=dst: e.activation(out=dst[:, i, :], in_=tf[:], func=AF.Sin, scale=TWO_PI * (1 - 1e-6)), ["tf"], [dk])
                        V(lambda e, i=i: e.tensor_scalar(out=dtab[:, i, :], in0=m0[:], scalar1=mag[:, i:i + 1], scalar2=None, op0=ALU.mult), ["m0", "mag"], ["dtab"])
                    zr = sb("zr", [128, 4096], F32, p3a); zi = sb("zi", [128, 4096], F32, p3a)
                    zr3 = zr[:].rearrange("p (i t) -> p i t", t=128); zi3 = zi[:].rearrange("p (i t) -> p i t", t=128)
                    tt_ = [sb("s5t%d" % i, [128, 512], BF16, p3a) for i in range(4)]
                    bu_r = [sb("bu_r%d" % i, [128, 512], BF16, p3a) for i in range(2)]; bu_i = [sb("bu_i%d" % i, [128, 512], BF16, p3a) for i in range(2)]
                    xr = sb("xr", [128, 32, 128], BF16, p3a); xi = sb("xi", [128, 32, 128], BF16, p3a)
                    xt1 = sb("sxt1", [128, 4096], BF16, p3a); xt2 = sb("sxt2", [128, 4096], BF16, p3a)
                    uch = [sb("uch%d" % i, [128, 8, 128], BF16, p3a) for i in range(2)]
                    cosF = cosT[:].rearrange("p i t -> p (i t)"); sinF = sinT[:].rearrange("p i t -> p (i t)"); dtF = dtab[:].rearrange("p i t -> p (i t)")
                    xrF = xr[:].rearrange("p i t -> p (i t)"); xiF = xi[:].rearrange("p i t -> p (i t)")
                    for ch in range(16):
                        uc = uch[ch % 2]; uk = "uch%d" % (ch % 2)
                        dma("sp", uc[:], s_u[:, :, ch * 128:(ch + 1) * 128], w=[uk])
                        for j in range(8):
                            pre, prek = kb.ps(); pim, pimk = kb.ps()
                            for r in range(4):
                                i = 4 * j + r
                                op("pe", lambda e, i=i, r=r: e.matmul(pre[:, r * 128:(r + 1) * 128], LBr[:, i, :], uc[:, j, :], start=True, stop=True), r=["LBr", uk], w=[prek], inc=(r == 3))
                            for r in range(4):
                                i = 4 * j + r
                                op("pe", lambda e, i=i, r=r: e.matmul(pim[:, r * 128:(r + 1) * 128], LBi[:, i, :], uc[:, j, :], start=True, stop=True), r=["LBi", uk], w=[pimk], inc=(r == 3))
                            sl = slice(j * 512, (j + 1) * 512)
                            br_ = bu_r[j % 2]; bi_ = bu_i[j % 2]; brk = "bu_r%d" % (j % 2); bik = "bu_i%d" % (j % 2)
                            A(lambda e: e.copy(out=br_[:], in_=pre[:]), [prek], [brk])
                            A(lambda e: e.copy(out=bi_[:], in_=pim[:]), [pimk], [bik])
                            V(lambda e: e.tensor_tensor(out=tt_[0][:], in0=br_[:], in1=cosF[:, sl], op=ALU.mult), [brk, "s5cos"], ["s5t0"])
                            V(lambda e: e.tensor_tensor(out=tt_[1][:], in0=bi_[:], in1=sinF[:, sl], op=ALU.mult), [bik, "s5sin"], ["s5t1"])
                            V(lambda e: e.tensor_tensor(out=tt_[2][:], in0=bi_[:], in1=cosF[:, sl], op=ALU.mult), [bik, "s5cos"], ["s5t2"])
                            V(lambda e: e.tensor_tensor(out=tt_[3][:], in0=br_[:], in1=sinF[:, sl], op=ALU.mult), [brk, "s5sin"], ["s5t3"])
                            op("pool", lambda e: e.tensor_tensor(out=zr[:, sl], in0=tt_[0][:], in1=tt_[1][:], op=ALU.add), r=["s5t0", "s5t1"], w=["zr"])
                            op("pool", lambda e: e.tensor_tensor(out=zi[:, sl], in0=tt_[2][:], in1=tt_[3][:], op=ALU.subtract), r=["s5t2", "s5t3"], w=["zi"])
                        if ch > 0:
                            V(lambda e: e.tensor_tensor(out=c_r[:], in0=mag[:], in1=ini_r[:], op=ALU.mult), ["mag", "ini_r"], ["c_r"])
                            V(lambda e: e.tensor_tensor(out=c_i[:], in0=mag[:], in1=ini_i[:], op=ALU.mult), ["mag", "ini_i"], ["c_i"])
                            V(lambda e: e.tensor_tensor(out=zr3[:, :, 0], in0=zr3[:, :, 0], in1=c_r[:], op=ALU.add), ["zr", "c_r"], ["zr"])
                            V(lambda e: e.tensor_tensor(out=zi3[:, :, 0], in0=zi3[:, :, 0], in1=c_i[:], op=ALU.add), ["zi", "c_i"], ["zi"])
                        V(lambda e: e.tensor_tensor_scan(out=zr[:], data0=dtF, data1=zr[:], initial=0.0, op0=ALU.mult, op1=ALU.add), ["zr", "dtab"], ["zr"])
                        V(lambda e: e.tensor_tensor_scan(out=zi[:], data0=dtF, data1=zi[:], initial=0.0, op0=ALU.mult, op1=ALU.add), ["zi", "dtab"], ["zi"])
                        if ch < 15:
                            V(lambda e: e.tensor_tensor(out=l_a[:], in0=e_c[:], in1=zr3[:, :, 127], op=ALU.mult), ["e_c", "zr"], ["l_a"])
                            V(lambda e: e.tensor_tensor(out=l_b[:], in0=e_s[:], in1=zi3[:, :, 127], op=ALU.mult), ["e_s", "zi"], ["l_b"])
                            V(lambda e: e.tensor_tensor(out=ini_r[:], in0=l_a[:], in1=l_b[:], op=ALU.subtract), ["l_a", "l_b"], ["ini_r"])
                            V(lambda e: e.tensor_tensor(out=l_a[:], in0=e_s[:], in1=zr3[:, :, 127], op=ALU.mult), ["e_s", "zr"], ["l_a"])
                            V(lambda e: e.tensor_tensor(out=l_b[:], in0=e_c[:], in1=zi3[:, :, 127], op=ALU.mult), ["e_c", "zi"], ["l_b"])
                            V(lambda e: e.tensor_tensor(out=ini_i[:], in0=l_a[:], in1=l_b[:], op=ALU.add), ["l_a", "l_b"], ["ini_i"])
                        if ch >= 8:
                            V(lambda e: e.tensor_tensor(out=xt1[:], in0=zr[:], in1=cosF, op=ALU.mult), ["zr", "s5cos"], ["sxt1"])
                            V(lambda e: e.tensor_tensor(out=xt2[:], in0=zi[:], in1=sinF, op=ALU.mult), ["zi", "s5sin"], ["sxt2"])
                            V(lambda e: e.tensor_tensor(out=xrF, in0=xt1[:], in1=xt2[:], op=ALU.subtract), ["sxt1", "sxt2"], ["xr"])
                            V(lambda e: e.tensor_tensor(out=xt1[:], in0=zr[:], in1=sinF, op=ALU.mult), ["zr", "s5sin", "xr"], ["sxt1"])
                            V(lambda e: e.tensor_tensor(out=xt2[:], in0=zi[:], in1=cosF, op=ALU.mult), ["zi", "s5cos", "xr"], ["sxt2"])
                            V(lambda e: e.tensor_tensor(out=xiF, in0=xt1[:], in1=xt2[:], op=ALU.add), ["sxt1", "sxt2"], ["xi"])
                            oc = ch - 8
                            for j4 in range(2):
                                py, pyk = kb.ps()
                                for jj in range(4):
                                    j = j4 * 4 + jj
                                    for r in range(4):
                                        i = 4 * j + r
                                        op("pe", lambda e, i=i, jj=jj, r=r: e.matmul(py[:, jj * 128:(jj + 1) * 128], W1[:, i, :], xr[:, i, :], start=(r == 0), stop=False), r=["W1", "xr"], w=[pyk], inc=False)
                                        op("pe", lambda e, i=i, jj=jj: e.matmul(py[:, jj * 128:(jj + 1) * 128], W2[:, i, :], xi[:, i, :], start=False, stop=False), r=["W2", "xi"], w=[pyk], inc=False)
                                    op("pe", lambda e, j=j, jj=jj: e.matmul(py[:, jj * 128:(jj + 1) * 128], Dg[:, j, :], uc[:, j, :], start=False, stop=True), r=["Dg", uk], w=[pyk], inc=(jj == 3))
                                A(lambda e, j4=j4: e.activation(out=yT[:, j4 * 4:(j4 + 1) * 4, oc * 128:(oc + 1) * 128], in_=py[:].rearrange("p (j t) -> p j t", j=4), func=AF.Gelu), [pyk], ["yT"])
                    kb.barrier()
                with ExitStack() as p3b:
                    wgl = sb("wgl", [128, 8, 1024], BF16, p3b); s5o = sb("s5o", [128, 8, NTOK], BF16, p3b)
                    sg = [sb("sg%d" % i, [128, 512], F32, p3b) for i in range(2)]
                    dma("pool", wgl[:], w_glu.rearrange("(k p) n -> p k n", p=128), w=["wgl"])
                    n = 0
                    for oc in range(8):
                        for tt in range(2):
                            sl = slice(tt * 512, (tt + 1) * 512)
                            pt, pk = kb.ps()
                            for k in range(8):
                                op("pe", lambda e, k=k: e.matmul(pt[:], wgl[:, k, oc * 128:(oc + 1) * 128], yT[:, k, sl], start=(k == 0), stop=(k == 7)), r=["wgl", "yT"], w=[pk], inc=(k == 7))
                            sgt = sg[n % 2]; sgk = "sg%d" % (n % 2); n += 1
                            op("act", lambda e: e.activation(out=sgt[:], in_=pt[:], func=AF.Sigmoid), r=[pk], w=[sgk])
                            op("dve", lambda e: e.tensor_tensor(out=s5o[:, oc, sl], in0=yT[:, oc, sl], in1=sgt[:], op=ALU.mult), r=["yT", sgk], w=["s5o"])
                    dma("sp", s_s5, s5o[:], r=["s5o"], key="st_s5")
                    kb.barrier()


        if stop_after >= 4:
            idx_all = sb("idx_all", [128, 64], I32); wk_all = sb("wk_all", [128, 64], F32)
            with ExitStack() as p4:
                merged = sb("merged", [128, 16, NTOK], BF16, p4)
                with ExitStack() as p4a:
                    hT = sb("m_hT", [128, 16, NTOK], BF16, p4a); at = sb("m_at", [128, 8, NTOK], BF16, p4a); s5 = sb("m_s5", [128, 8, NTOK], BF16, p4a)
                    dma("sp", hT[:], s_hT, w=["m_hT"]); dma("sp", at[:], s_attn, w=["m_at"]); dma("sp", s5[:], s_s5, w=["m_s5"])
                    wbm = [sb("wbm%d" % i, [128, 8, 256], BF16, p4a) for i in range(2)]; wbs = [sb("wbs%d" % i, [128, 8, 256], BF16, p4a) for i in range(2)]
                    wgm = [sb("wgm%d" % i, [128, 16, 256], BF16, p4a) for i in range(2)]; wgs = [sb("wgs%d" % i, [128, 16, 256], BF16, p4a) for i in range(2)]
                    sgm = sb("sgm", [128, 512], F32, p4a); sgs = sb("sgs", [128, 512], F32, p4a); mt1 = sb("mt1", [128, 512], F32, p4a); mt2 = sb("mt2", [128, 512], F32, p4a)
                    wbm_v = w_br_mla.rearrange("(k p) n -> p k n", p=128); wbs_v = w_br_s5.rearrange("(k p) n -> p k n", p=128)
                    for cb in range(8):
                        i = cb % 2; c0 = cb * 256
                        dma("pool", wbm[i][:], wbm_v[:, :, c0:c0 + 256], w=["wbm%d" % i]); dma("pool", wbs[i][:], wbs_v[:, :, c0:c0 + 256], w=["wbs%d" % i])
                        dma("pool", wgm[i][:], w_in_v[:, :, 2112 + c0:2112 + c0 + 256], w=["wgm%d" % i]); dma("pool", wgs[i][:], w_in_v[:, :, 4160 + c0:4160 + c0 + 256], w=["wgs%d" % i])
                        for f in range(2):
                            fc = cb * 2 + f; fs = slice(f * 128, (f + 1) * 128)
                            for tt in range(2):
                                sl = slice(tt * 512, (tt + 1) * 512)
                                p1_, k1 = kb.ps(); p2_, k2 = kb.ps(); p3_, k3 = kb.ps(); p4_, k4 = kb.ps()
                                for k in range(8):
                                    op("pe", lambda e, k=k: e.matmul(p1_[:], wbm[i][:, k, fs], at[:, k, sl], start=(k == 0), stop=(k == 7)), r=["wbm%d" % i, "m_at"], w=[k1], inc=(k == 7))
                                for k in range(8):
                                    op("pe", lambda e, k=k: e.matmul(p2_[:], wbs[i][:, k, fs], s5[:, k, sl], start=(k == 0), stop=(k == 7)), r=["wbs%d" % i, "m_s5"], w=[k2], inc=(k == 7))
                                for k in range(16):
                                    op("pe", lambda e, k=k: e.matmul(p3_[:], wgm[i][:, k, fs], hT[:, k, sl], start=(k == 0), stop=(k == 15)), r=["wgm%d" % i, "m_hT"], w=[k3], inc=(k == 15))
                                for k in range(16):
                                    op("pe", lambda e, k=k: e.matmul(p4_[:], wgs[i][:, k, fs], hT[:, k, sl], start=(k == 0), stop=(k == 15)), r=["wgs%d" % i, "m_hT"], w=[k4], inc=(k == 15))
                                op("act", lambda e: e.activation(out=sgm[:], in_=p3_[:], func=AF.Sigmoid), r=[k3], w=["sgm"])
                                op("act", lambda e: e.activation(out=sgs[:], in_=p4_[:], func=AF.Sigmoid), r=[k4], w=["sgs"])
                                op("dve", lambda e: e.tensor_tensor(out=mt1[:], in0=p1_[:], in1=sgm[:], op=ALU.mult), r=[k1, "sgm"], w=["mt1"])
                                op("dve", lambda e: e.tensor_tensor(out=mt2[:], in0=p2_[:], in1=sgs[:], op=ALU.mult), r=[k2, "sgs"], w=["mt2"])
                                op("pool", lambda e: e.tensor_tensor(out=merged[:, fc, sl], in0=mt1[:], in1=mt2[:], op=ALU.add), r=["mt1", "mt2"], w=["merged"])
                    kb.barrier()
                with ExitStack() as p4b:
                    wo = sb("wo", [128, 16, D], BF16, p4b); wr = sb("wr", [128, 16, NE], BF16, p4b)
                    for q in range(4):
                        dma("pool", wo[:, :, q * 512:(q + 1) * 512], w_out.rearrange("(k p) n -> p k n", p=128)[:, :, q * 512:(q + 1) * 512], w=["wo"], key="wo%d" % q)
                    dma("pool", wr[:], w_router.rearrange("(k p) n -> p k n", p=128), w=["wr"])
                    gbc = sb("gbc", [128, D], F32, p4b)
                    build_bc(gbc, "gbc", 2, p4b); build_bc(sbc, "sbc", 3, p4b); build_bc(shbc, "shbc", 4, p4b)
                    h2Ts = [sb("h2T%d" % i, [128, 16, 128], BF16, p4b) for i in range(2)]
                    xts = [sb("b_xt%d" % i, [128, D], F32, p4b) for i in range(2)]; x1s = [sb("b_x1%d" % i, [128, D], F32, p4b) for i in range(2)]
                    junk = sb("b_junk", [128, D], BF16, p4b); junk2 = sb("b_junk2", [128, D], BF16, p4b); ss = sb("b_ss", [128, 1], F32, p4b); rstd = sb("b_rstd", [128, 1], F32, p4b)
                    ss4 = sb("b_ss4", [128, 4], F32, p4b); rs1 = sb("b_rs1", [128, 1], F32, p4b)
                    tmp = sb("b_tmp", [128, D], F32, p4b); tmp2 = sb("b_tmp2", [128, D], F32, p4b); htoks = [sb("b_htok%d" % i, [128, D], BF16, p4b) for i in range(2)]
                    rb = sb("rbias", [128, NE], F32, p4b); ecap = sb("ecap", [128, NE], F32, p4b); ltri_f = sb("ltri_f", [128, 128], F32, p4b); ltri = sb("ltri", [128, 128], BF16, p4b)
                    maskall = sb("maskall", [128, 8, NE], BF16, p4b)
                    dma("sp", rb[:], rbias_in.partition_broadcast(128), w=["rbias"]); dma("sp", ecap[:], ecap_in.partition_broadcast(128), w=["ecap"]); dma("sp", ltri_f[:], ltri_in, w=["ltri_f"])
                    op("dve", lambda e: e.tensor_copy(out=ltri[:], in_=ltri_f[:]), r=["ltri_f"], w=["ltri"])
                    R = {}
                    for nm, shp in (("sc", [128, NE]), ("sel", [128, NE]), ("selm", [128, NE]), ("m8g", [128, 8, 8]), ("gs", [128, 8]), ("gtop", [128, 8]), ("gmask", [128, 8]),
                                    ("pen", [128, 8]), ("top8", [128, 8]), ("tmask", [128, NE]), ("wraw", [128, NE]), ("wsum", [128, 1]), ("wfull", [128, NE]),
                                    ("ridx", [128, NE]), ("okm", [128, NE]), ("okt", [128, NE]), ("oh", [128, NE]), ("ot", [128, NE]), ("idxf", [128, 8])):
                        R[nm] = sb("r_" + nm, shp, F32, p4b)
                    V = lambda f, r, w: op("dve", f, r=r, w=w)
                    def stage_A(tb):
                        i = tb % 2
                        xt = xts[i]; xtk = "b_xt%d" % i; x1 = x1s[i]; x1k = "b_x1%d" % i
                        h2T = h2Ts[i]; h2k = "h2T%d" % i
                        dma("sp", xt[:], x_own[tb * 128:(tb + 1) * 128, :], w=[xtk])
                        pts = []
                        for nb in range(4):
                            pt, pk = kb.ps(); pts.append((pt, pk))
                            for k in range(16):
                                op("pe", lambda e, k=k: e.matmul(pt[:], merged[:, k, tb * 128:(tb + 1) * 128], wo[:, k, nb * 512:(nb + 1) * 512], start=(k == 0), stop=(k == 15)), r=["merged", "wo"], w=[pk], inc=(k == 15))
                            op("act", lambda e, nb=nb, pt=pt: e.activation(out=junk[:, nb * 512:(nb + 1) * 512], in_=pt[:], func=AF.Square, accum_out=ss4[:, nb:nb + 1]), r=[pk], w=["b_junk", "b_ss4"])
                        V(lambda e: e.tensor_reduce(out=rs1[:], in_=ss4[:], axis=AX.X, op=ALU.add), ["b_ss4"], ["b_rs1"])
                        op("act", lambda e: e.activation(out=rs1[:], in_=rs1[:], func=AF.Sqrt, bias=epsc[:], scale=1.0 / D), r=["b_rs1", "epsc"], w=["b_rs1"])
                        V(lambda e: e.reciprocal(out=rs1[:], in_=rs1[:]), ["b_rs1"], ["b_rs1"])
                        for nb in range(4):
                            pt, pk = pts[nb]; cs_ = slice(nb * 512, (nb + 1) * 512)
                            V(lambda e, pt=pt, cs_=cs_: e.tensor_tensor(out=tmp2[:, cs_], in0=pt[:], in1=gbc[:, cs_], op=ALU.mult), [pk, "gbc"], ["b_tmp2"])
                        V(lambda e: e.scalar_tensor_tensor(out=x1[:], in0=tmp2[:], scalar=rs1[:, 0:1], in1=xt[:], op0=ALU.mult, op1=ALU.add), ["b_tmp2", "b_rs1", xtk], [x1k])
                        dma("sp", s_x1[tb * 128:(tb + 1) * 128, :], x1[:], r=[x1k], key="st_x1")

                    def stage_A2(tb):
                        i = tb % 2
                        x1 = x1s[i]; x1k = "b_x1%d" % i
                        h2T = h2Ts[i]; h2k = "h2T%d" % i
                        prenorm_block((x1, x1k, junk2, ss, rstd, tmp, htoks[i], "b_htok%d" % i), None, False, 0, h2T, h2k, names=("b_junk2", "b_ss", "b_rstd", "b_tmp"))
                        dma("sp", s_h2T[:, :, tb * 128:(tb + 1) * 128], h2T[:], r=[h2k], key="st_h2T")

                    def stage_B(tb):
                        i = tb % 2
                        h2T = h2Ts[i]; h2k = "h2T%d" % i
                        pl, plk = kb.ps()
                        for k in range(16):
                            op("pe", lambda e, k=k: e.matmul(pl[:, 0:NE], h2T[:, k, :], wr[:, k, :], start=(k == 0), stop=(k == 15)), r=[h2k, "wr"], w=[plk], inc=(k == 15))
                        op("act", lambda e: e.activation(out=R["sc"][:], in_=pl[:, 0:NE], func=AF.Sigmoid), r=[plk], w=["r_sc"])
                        V(lambda e: e.tensor_tensor(out=R["sel"][:], in0=R["sc"][:], in1=rb[:], op=ALU.add), ["r_sc", "rbias"], ["r_sel"])
                        sel3 = R["sel"][:].rearrange("p (g e) -> p g e", g=8)
                        for g in range(8):
                            V(lambda e, g=g: e.max(out=R["m8g"][:, g, :], in_=sel3[:, g, :]), ["r_sel"], ["r_m8g"])
                        V(lambda e: e.tensor_tensor(out=R["gs"][:], in0=R["m8g"][:, :, 0], in1=R["m8g"][:, :, 1], op=ALU.add), ["r_m8g"], ["r_gs"])
                        V(lambda e: e.max(out=R["gtop"][:], in_=R["gs"][:]), ["r_gs"], ["r_gtop"])
                        V(lambda e: e.tensor_scalar(out=R["gmask"][:], in0=R["gs"][:], scalar1=R["gtop"][:, 3:4], scalar2=None, op0=ALU.is_ge), ["r_gs", "r_gtop"], ["r_gmask"])
                        V(lambda e: e.tensor_scalar(out=R["pen"][:], in0=R["gmask"][:], scalar1=-1.0, scalar2=1e9, op0=ALU.add, op1=ALU.mult), ["r_gmask"], ["r_pen"])
                        selm3 = R["selm"][:].rearrange("p (g e) -> p g e", g=8)
                        for g in range(8):
                            V(lambda e, g=g: e.tensor_scalar(out=selm3[:, g, :], in0=sel3[:, g, :], scalar1=R["pen"][:, g:g + 1], scalar2=None, op0=ALU.add), ["r_sel", "r_pen"], ["r_selm"])
                        V(lambda e: e.max(out=R["top8"][:], in_=R["selm"][:]), ["r_selm"], ["r_top8"])
                        V(lambda e: e.tensor_scalar(out=R["tmask"][:], in0=R["selm"][:], scalar1=R["top8"][:, 5:6], scalar2=None, op0=ALU.is_ge), ["r_selm", "r_top8"], ["r_tmask"])
                        V(lambda e: e.tensor_copy(out=maskall[:, tb, :], in_=R["tmask"][:]), ["r_tmask"], ["maskall"])
                        V(lambda e: e.tensor_tensor(out=R["wraw"][:], in0=R["sc"][:], in1=R["tmask"][:], op=ALU.mult), ["r_sc", "r_tmask"], ["r_wraw"])
                        V(lambda e: e.tensor_reduce(out=R["wsum"][:], in_=R["wraw"][:], axis=AX.X, op=ALU.add), ["r_wraw"], ["r_wsum"])
                        V(lambda e: e.reciprocal(out=R["wsum"][:], in_=R["wsum"][:]), ["r_wsum"], ["r_wsum"])
                        V(lambda e: e.tensor_scalar(out=R["wfull"][:], in0=R["wraw"][:], scalar1=R["wsum"][:, 0:1], scalar2=2.5, op0=ALU.mult, op1=ALU.mult), ["r_wraw", "r_wsum"], ["r_wfull"])
                        pc, pck = kb.ps()
                        for t2 in range(tb + 1):
                            lt = ltri if t2 == tb else ones_b
                            op("pe", lambda e, t2=t2, lt=lt: e.matmul(pc[:, 0:NE], lt[:], maskall[:, t2, :], start=(t2 == 0), stop=(t2 == tb)), r=["ltri", "ones_b", "maskall"], w=[pck], inc=(t2 == tb))
                        V(lambda e: e.tensor_tensor(out=R["ridx"][:], in0=pc[:, 0:NE], in1=ecap[:], op=ALU.add), [pck, "ecap"], ["r_ridx"])
                        V(lambda e: e.tensor_scalar(out=R["okm"][:], in0=pc[:, 0:NE], scalar1=CAP - 0.5, scalar2=None, op0=ALU.is_lt), [pck], ["r_okm"])
                        V(lambda e: e.tensor_scalar(out=R["okt"][:], in0=R["okm"][:], scalar1=-1.0, scalar2=-1.0e6, op0=ALU.add, op1=ALU.mult), ["r_okm"], ["r_okt"])
                        V(lambda e: e.tensor_tensor(out=R["ridx"][:], in0=R["ridx"][:], in1=R["okm"][:], op=ALU.mult), ["r_ridx", "r_okm"], ["r_ridx"])
                        V(lambda e: e.tensor_tensor(out=R["ridx"][:], in0=R["ridx"][:], in1=R["okt"][:], op=ALU.add), ["r_ridx", "r_okt"], ["r_ridx"])
                        for k in range(6):
                            V(lambda e, k=k: e.tensor_scalar(out=R["oh"][:], in0=R["selm"][:], scalar1=R["top8"][:, k:k + 1], scalar2=None, op0=ALU.is_equal), ["r_selm", "r_top8"], ["r_oh"])
                            V(lambda e: e.tensor_tensor(out=R["ot"][:], in0=R["oh"][:], in1=R["ridx"][:], op=ALU.mult), ["r_oh", "r_ridx"], ["r_ot"])
                            V(lambda e, k=k: e.tensor_reduce(out=R["idxf"][:, k:k + 1], in_=R["ot"][:], axis=AX.X, op=ALU.add), ["r_ot"], ["r_idxf"])
                            V(lambda e: e.tensor_tensor(out=R["ot"][:], in0=R["oh"][:], in1=R["wfull"][:], op=ALU.mult), ["r_oh", "r_wfull"], ["r_ot"])
                            V(lambda e, k=k: e.tensor_reduce(out=wk_all[:, tb * 8 + k:tb * 8 + k + 1], in_=R["ot"][:], axis=AX.X, op=ALU.add), ["r_ot"], ["wk_all"])
                        V(lambda e: e.tensor_copy(out=idx_all[:, tb * 8:tb * 8 + 6], in_=R["idxf"][:, 0:6]), ["r_idxf"], ["idx_all"])
                        for k in range(6):
                            kb.idma(s_xe[:, :], bass.IndirectOffsetOnAxis(ap=idx_all[:, tb * 8 + k:tb * 8 + k + 1], axis=0), htoks[i][:, :], None, NE * CAP - 1,
                                    r=["idx_all", "b_htok%d" % i], key="sc_xe%d" % (k % 2))

                    stage_A(0); stage_A(1); stage_A2(0)
                    for tb in range(8):
                        if tb + 2 < 8:
                            stage_A(tb + 2)
                        if tb + 1 < 8:
                            stage_A2(tb + 1)
                        stage_B(tb)
                    if "s_idx" in dbg:
                        dma("sp", s_idx, idx_all[:], r=["idx_all"], key="dbg_idx")
                    kb.barrier()

        if stop_after >= 5:
            with ExitStack() as p5a:
                h2T = sb("e_h2T", [128, 16, NTOK], BF16, p5a)
                dma("sp", h2T[:], s_h2T, w=["e_h2T"])
                wsg = sb("wsg", [128, 16, 512], BF16, p5a); wsu = sb("wsu", [128, 16, 512], BF16, p5a); wsd = sb("wsd", [128, 4, D], BF16, p5a)
                dma("pool", wsg[:], w_sg.rearrange("(k p) n -> p k n", p=128), w=["wsg"]); dma("pool", wsu[:], w_su.rearrange("(k p) n -> p k n", p=128), w=["wsu"])
                dma("pool", wsd[:], w_sd.rearrange("(k p) n -> p k n", p=128), w=["wsd"])
                actS = sb("actS", [128, 4, NTOK], BF16, p5a); sgl = sb("sh_sgl", [128, 512], F32, p5a); ysh = [sb("ysh%d" % i, [128, D], F32, p5a) for i in range(2)]
                for c in range(4):
                    for tt in range(2):
                        sl = slice(tt * 512, (tt + 1) * 512)
                        pg, pgk = kb.ps(); pu, puk = kb.ps()
                        for k in range(16):
                            op("pe", lambda e, k=k: e.matmul(pg[:], wsg[:, k, c * 128:(c + 1) * 128], h2T[:, k, sl], start=(k == 0), stop=(k == 15)), r=["wsg", "e_h2T"], w=[pgk], inc=(k == 15))
                        for k in range(16):
                            op("pe", lambda e, k=k: e.matmul(pu[:], wsu[:, k, c * 128:(c + 1) * 128], h2T[:, k, sl], start=(k == 0), stop=(k == 15)), r=["wsu", "e_h2T"], w=[puk], inc=(k == 15))
                        op("act", lambda e: e.activation(out=sgl[:], in_=pg[:], func=AF.Silu), r=[pgk], w=["sh_sgl"])
                        op("dve", lambda e: e.tensor_tensor(out=actS[:, c, sl], in0=pu[:], in1=sgl[:], op=ALU.mult), r=[puk, "sh_sgl"], w=["actS"])
                for tb in range(8):
                    yt = ysh[tb % 2]; ytk = "ysh%d" % (tb % 2)
                    for nb in range(4):
                        pt, pk = kb.ps()
                        for k in range(4):
                            op("pe", lambda e, k=k: e.matmul(pt[:], actS[:, k, tb * 128:(tb + 1) * 128], wsd[:, k, nb * 512:(nb + 1) * 512], start=(k == 0), stop=(k == 3)), r=["actS", "wsd"], w=[pk], inc=(k == 3))
                        if nb % 2 == 0:
                            op("act", lambda e, pt=pt, nb=nb: e.copy(out=yt[:, nb * 512:(nb + 1) * 512], in_=pt[:]), r=[pk], w=[ytk])
                        else:
                            op("dve", lambda e, pt=pt, nb=nb: e.tensor_copy(out=yt[:, nb * 512:(nb + 1) * 512], in_=pt[:]), r=[pk], w=[ytk])
                    dma("sp", s_sh[tb * 128:(tb + 1) * 128, :], yt[:], r=[ytk], key="st_sh")
                kb.barrier()
            NSB = CAP // 128
            with ExitStack() as p5b:
                wgs_ = [sb("xwg%d" % i, [128, 16, 512], BF16, p5b) for i in range(2)]; wus_ = [sb("xwu%d" % i, [128, 16, 512], BF16, p5b) for i in range(2)]
                wds_ = [sb("xwd%d" % i, [128, 4, D], BF16, p5b) for i in range(2)]
                xtok = [[sb("xtok%d_%d" % (i, j), [128, D], BF16, p5b) for j in range(NSB)] for i in range(2)]
                xeTs = [sb("xeT%d" % i, [128, 16, CAP], BF16, p5b) for i in range(2)]
                actE = sb("actE", [128, 4, CAP], BF16, p5b); sges = [sb("sge%d" % i, [128, CAP], F32, p5b) for i in range(2)]
                yes = [sb("ye%d" % i, [128, D], BF16, p5b) for i in range(3)]
                cnt = {"ny": 0, "ns": 0}

                def issue_loads(ex):
                    i = ex % 2
                    dma("pool", wgs_[i][:], w_eg[ex].rearrange("(k p) n -> p k n", p=128), w=["xwg%d" % i])
                    dma("pool", wus_[i][:], w_eu[ex].rearrange("(k p) n -> p k n", p=128), w=["xwu%d" % i])
                    dma("pool", wds_[i][:], w_ed[ex].rearrange("(k p) n -> p k n", p=128), w=["xwd%d" % i])
                    for sbk in range(NSB):
                        r0 = ex * CAP + sbk * 128
                        dma("sp", xtok[i][sbk][:], s_xe[r0:r0 + 128, :], w=["xtok%d_%d" % (i, sbk)])

                def emit_T(ex):
                    i = ex % 2
                    xeT = xeTs[i]; xk = "xeT%d" % i
                    for sbk in range(NSB):
                        xk_ = xtok[i][sbk]; xkk = "xtok%d_%d" % (i, sbk)
                        for half in range(2):
                            pt, pk = kb.pb()
                            for j in range(8):
                                fc = half * 8 + j
                                op("pe", lambda e, j=j, fc=fc: e.transpose(pt[:, j * 128:(j + 1) * 128], xk_[:, fc * 128:(fc + 1) * 128], ident_b[:]), r=[xkk, "ident_b"], w=[pk], inc=(j == 7))
                            if half == 0:
                                op("act", lambda e: e.copy(out=xeT[:, 0:8, sbk * 128:(sbk + 1) * 128], in_=pt[:].rearrange("p (c t) -> p c t", c=8)), r=[pk], w=[xk])
                            else:
                                op("dve", lambda e: e.tensor_copy(out=xeT[:, 8:16, sbk * 128:(sbk + 1) * 128], in_=pt[:].rearrange("p (c t) -> p c t", c=8)), r=[pk], w=[xk])

                def emit_GU(ex):
                    i = ex % 2
                    xeT = xeTs[i]; xk = "xeT%d" % i
                    for c in range(4):
                        pg, pgk = kb.ps(); pu, puk = kb.ps()
                        for k in range(16):
                            op("pe", lambda e, k=k: e.matmul(pg[:, 0:CAP], wgs_[i][:, k, c * 128:(c + 1) * 128], xeT[:, k, :], start=(k == 0), stop=(k == 15)), r=["xwg%d" % i, xk], w=[pgk], inc=(k == 15))
                        for k in range(16):
                            op("pe", lambda e, k=k: e.matmul(pu[:, 0:CAP], wus_[i][:, k, c * 128:(c + 1) * 128], xeT[:, k, :], start=(k == 0), stop=(k == 15)), r=["xwu%d" % i, xk], w=[puk], inc=(k == 15))
                        sge = sges[cnt["ns"] % 2]; sgk = "sge%d" % (cnt["ns"] % 2); cnt["ns"] += 1
                        op("act", lambda e: e.activation(out=sge[:], in_=pg[:, 0:CAP], func=AF.Silu), r=[pgk], w=[sgk])
                        op("dve", lambda e: e.tensor_tensor(out=actE[:, c, :], in0=pu[:, 0:CAP], in1=sge[:], op=ALU.mult), r=[puk, sgk], w=["actE"])

                def emit_DN(ex):
                    i = ex % 2
                    for sbk in range(NSB):
                        yt = yes[cnt["ny"] % 3]; ytk = "ye%d" % (cnt["ny"] % 3); cnt["ny"] += 1
                        for nb in range(4):
                            pt, pk = kb.ps()
                            for k in range(4):
                                op("pe", lambda e, k=k: e.matmul(pt[:], actE[:, k, sbk * 128:(sbk + 1) * 128], wds_[i][:, k, nb * 512:(nb + 1) * 512], start=(k == 0), stop=(k == 3)), r=["actE", "xwd%d" % i], w=[pk], inc=(k == 3))
                            if nb % 2 == 0:
                                op("act", lambda e, pt=pt, nb=nb: e.copy(out=yt[:, nb * 512:(nb + 1) * 512], in_=pt[:]), r=[pk], w=[ytk])
                            else:
                                op("dve", lambda e, pt=pt, nb=nb: e.tensor_copy(out=yt[:, nb * 512:(nb + 1) * 512], in_=pt[:]), r=[pk], w=[ytk])
                        r0 = ex * CAP + sbk * 128
                        dma("sp", s_ye[r0:r0 + 128, :], yt[:], r=[ytk], key="st_" + ytk)

                issue_loads(0); issue_loads(1)
                emit_T(0)
                for ex in range(NE):
                    emit_GU(ex)
                    if ex + 1 < NE:
                        emit_T(ex + 1)
                    emit_DN(ex)
                    if ex + 2 < NE:
                        issue_loads(ex + 2)
                kb.barrier()
            with ExitStack() as p5c:
                gbc2 = sb("gbc2", [128, D], F32, p5c)
                build_bc(gbc2, "gbc2", 5, p5c)
                accs = [sb("acc%d" % i, [128, D], F32, p5c) for i in range(2)]; gts = [sb("gt%d" % i, [128, D], BF16, p5c) for i in range(12)]
                x1s = [sb("c_x1%d" % i, [128, D], F32, p5c) for i in range(2)]; cj = sb("c_junk", [128, D], BF16, p5c); css = sb("c_ss", [128, 1], F32, p5c)
                ctmp = sb("c_tmp", [128, D], F32, p5c); outs = [sb("c_out%d" % i, [128, D], F32, p5c) for i in range(2)]
                ng = 0
                for tb in range(8):
                    i = tb % 2
                    acc = accs[i]; acck = "acc%d" % i
                    dma("sp", acc[:], s_sh[tb * 128:(tb + 1) * 128, :], w=[acck])
                    dma("sp", x1s[i][:], s_x1[tb * 128:(tb + 1) * 128, :], w=["c_x1%d" % i])
                    for k in range(6):
                        gt = gts[i * 6 + k]; gtk = "gt%d" % (i * 6 + k)
                        kb.idma(gt[:, :], None, s_ye[:, :], bass.IndirectOffsetOnAxis(ap=idx_all[:, tb * 8 + k:tb * 8 + k + 1], axis=0), NE * CAP - 1, r=["idx_all"], w=[gtk], key="ga_" + gtk)
                    for k in range(6):
                        gt = gts[i * 6 + k]; gtk = "gt%d" % (i * 6 + k)
                        op("dve", lambda e, k=k: e.scalar_tensor_tensor(out=acc[:], in0=gt[:], scalar=wk_all[:, tb * 8 + k:tb * 8 + k + 1], in1=acc[:], op0=ALU.mult, op1=ALU.add), r=[gtk, "wk_all", acck], w=[acck])
                    op("act", lambda e: e.activation(out=cj[:], in_=acc[:], func=AF.Square, accum_out=css[:]), r=[acck], w=["c_junk", "c_ss"])
                    op("act", lambda e: e.activation(out=css[:], in_=css[:], func=AF.Sqrt, bias=epsc[:], scale=1.0 / D), r=["c_ss", "epsc"], w=["c_ss"])
                    op("dve", lambda e: e.reciprocal(out=css[:], in_=css[:]), r=["c_ss"], w=["c_ss"])
                    op("dve", lambda e: e.tensor_tensor(out=ctmp[:], in0=acc[:], in1=gbc2[:], op=ALU.mult), r=[acck, "gbc2"], w=["c_tmp"])
                    op("dve", lambda e: e.scalar_tensor_tensor(out=outs[i][:], in0=ctmp[:], scalar=css[:, 0:1], in1=x1s[i][:], op0=ALU.mult, op1=ALU.add), r=["c_tmp", "c_ss", "c_x1%d" % i], w=["c_out%d" % i])
                    dma("sp", out_d[tb * 128:(tb + 1) * 128, :], outs[i][:], r=["c_out%d" % i], key="st_out")
                kb.barrier()

        kb.barrier()
    return nc


def host_inputs(inputs, core):
    b, half = core // 2, core % 2
    f32 = np.float32
    x = np.asarray(inputs["x"], f32)
    pos = np.asarray(inputs["positions"]).astype(np.int32)
    own = slice(half * NTOK, (half + 1) * NTOK)
    m = {}
    m["x_own"] = np.ascontiguousarray(x[b, own])
    if half == 1:
        m["x_ctx"] = np.ascontiguousarray(x[b, 0:NTOK])
        pctx = pos[b, 0:NTOK]
    else:
        m["x_ctx"] = np.ascontiguousarray(x[b, own])
        pctx = np.full((NTOK,), 1 << 22, np.int32)
    pext = np.concatenate([pctx, pos[b, own]]).astype(np.int32)
    m["pos_row"] = pext.reshape(1, NEXT)
    m["pos_col"] = np.ascontiguousarray(pext.reshape(16, 128).T)
    m["c_col"] = np.ascontiguousarray(np.asarray(inputs["c"], f32)[b].reshape(16, 128).T)
    m["flag"] = np.full((128, 1), float(half), f32)
    m["ident"] = np.eye(128, dtype=f32)
    invf = (10000.0 ** (-np.arange(32, dtype=np.float32) / 32.0)).astype(f32)
    m["invf"] = (np.concatenate([invf, invf]) / f32(TWO_PI)).astype(f32).reshape(64, 1)
    m["iota128"] = np.tile(np.arange(128, dtype=f32)[None, :], (128, 1))
    m["b_ada"] = np.asarray(inputs["b_ada"], f32).reshape(1, 6 * D)
    gs = [np.asarray(inputs[k], f32)[0].reshape(16, 128).T for k in ("g_pre_mix", "g_post_mix", "g_pre_ffn", "g_post_ffn")]
    m["gvec"] = np.ascontiguousarray(np.concatenate(gs, axis=1))
    m["gq"] = np.ascontiguousarray(np.asarray(inputs["g_q"], f32)[0].reshape(4, 128).T)
    m["gkv"] = np.ascontiguousarray(np.asarray(inputs["g_kv"], f32)[0].reshape(4, 128).T)
    m["w_ada"] = np.asarray(inputs["w_ada"], f32)[0]
    m["w_in"] = np.asarray(inputs["w_in"], f32)[0]
    for k in ("w_uq", "w_uk", "w_uv", "w_glu", "w_br_mla", "w_br_s5", "w_out", "w_router", "w_exp_gate", "w_exp_up", "w_exp_down", "w_sh_gate", "w_sh_up", "w_sh_down"):
        m[k] = np.asarray(inputs[k], f32)[0]
    m["router_bias"] = np.asarray(inputs["router_bias"], f32).reshape(1, NE)
    m["ecap"] = (np.arange(NE, dtype=f32) * CAP).reshape(1, NE)
    m["ltri"] = np.triu(np.ones((128, 128), f32), 1)

    def state_layout(a):
        a = np.asarray(a, f32)
        r = a.reshape((32, 2) + a.shape[1:])
        r = np.moveaxis(r, 0, 2)
        return np.ascontiguousarray(r.reshape((128, 32) + a.shape[2:]))
    m["a_re_sl"] = state_layout(inputs["a_re"][0]); m["a_im_sl"] = state_layout(inputs["a_im"][0])
    m["ldt2"] = np.ascontiguousarray(np.asarray(inputs["log_dt"], f32)[0].reshape(32, 2).T)
    m["b_re_sl"] = state_layout(inputs["b_re"][0]); m["b_im_sl"] = state_layout(inputs["b_im"][0])
    m["c_re_sl"] = state_layout(np.swapaxes(np.asarray(inputs["c_re"], f32)[0], 1, 2))
    m["c_im_sl"] = state_layout(np.swapaxes(np.asarray(inputs["c_im"], f32)[0], 1, 2))
    m["dcol"] = np.ascontiguousarray(np.asarray(inputs["d_skip"], f32)[0].reshape(8, 128).T)
    return m


def kernel(**inputs):
    nc = build_program()
    in_maps = [host_inputs(inputs, c) for c in range(8)]
    res = run_bass_kernel_spmd(nc, in_maps, core_ids=list(range(8)))
    out = np.zeros((4, 2048, D), np.float32)
    for c in range(8):
        out[c // 2, (c % 2) * NTOK:(c % 2 + 1) * NTOK] = res.results[c]["out"]
    return out
```

```python
import math
from contextlib import ExitStack
import numpy as np
import concourse.bass as bass
import concourse.mybir as mybir
from concourse.bass_utils import run_bass_kernel_spmd

F32 = mybir.dt.float32
BF16 = mybir.dt.bfloat16
I32 = mybir.dt.int32
ALU = mybir.AluOpType
AF = mybir.ActivationFunctionType
AX = mybir.AxisListType

D = 2048
NTOK = 1024
NEXT = 2048
EPS = 1e-6
NE = 64
CAP = 384
TWO_PI = 2.0 * math.pi


class KB:
    def __init__(self, nc, es):
        self.nc = nc
        self.es = es
        self.E = {"pe": nc.tensor, "act": nc.scalar, "dve": nc.vector, "pool": nc.gpsimd, "sp": nc.sync}
        self.esem = {e: es.enter_context(nc.semaphore("s_" + e)) for e in self.E}
        self.ecnt = {e: 0 for e in self.E}
        self.seen = {e: {} for e in self.E}
        self.lastw = {}
        self.readers = {}
        self.dsem = {}
        self.pool = []
        self.bnd_reg = None
        self.nsem = 0
        self.nps = 0
        self.ps_ring = [0, 1, 2, 3, 4, 5]
        self.npb = 0

    def _wait(self, eng, tok):
        key, sem, val = tok
        if eng == "pe" and key == "e:pe":
            return
        if self.seen[eng].get(key, 0) >= val:
            return
        self.E[eng].wait_ge(sem, val)
        self.seen[eng][key] = val

    def _deps(self, eng, r, w):
        toks = []
        for b in r:
            t = self.lastw.get(b)
            if t is not None:
                toks.append(t)
        for b in w:
            t = self.lastw.get(b)
            if t is not None:
                toks.append(t)
            toks.extend(self.readers.get(b, {}).values())
        for t in toks:
            self._wait(eng, t)

    def _update(self, tok, r, w):
        for b in r:
            d = self.readers.setdefault(b, {})
            old = d.get(tok[0])
            if old is None or old[2] < tok[2]:
                d[tok[0]] = tok
        for b in w:
            self.lastw[b] = tok
            self.readers[b] = {}

    def op(self, eng, fn, r=(), w=(), inc=True):
        self._deps(eng, r, w)
        inst = fn(self.E[eng])
        if inc:
            self.ecnt[eng] += 1
            inst.then_inc(self.esem[eng], 1)
            tok = ("e:" + eng, self.esem[eng], self.ecnt[eng])
        else:
            tok = ("e:" + eng, self.esem[eng], self.ecnt[eng] + 1)
        self._update(tok, r, w)
        return inst

    def _ent(self, key):
        if key not in self.dsem:
            if self.pool:
                self.dsem[key] = self.pool.pop()
            else:
                self.nsem += 1
                self.dsem[key] = [self.es.enter_context(self.nc.semaphore("dq%d" % self.nsem)), 0, self.nsem]
        return self.dsem[key]

    def dma(self, q, out, in_, r=(), w=(), key=None, **kw):
        self._deps(q, r, w)
        if key is None:
            key = str(w[0] if w else r[0])
        ent = self._ent(key)
        inst = self.E[q].dma_start(out=out, in_=in_, **kw)
        ent[1] += 16
        inst.then_inc(ent[0], 16)
        tok = ("d:%d" % ent[2], ent[0], ent[1])
        self._update(tok, r, w)
        return inst

    def idma(self, out, out_off, in_, in_off, bound, r=(), w=(), key=None):
        q = "pool"
        self._deps(q, r, w)
        ent = self._ent(key)
        if self.bnd_reg is None:
            self.bnd_reg = self.nc.gpsimd.alloc_register("bnd")
            self.nc.gpsimd.reg_mov(self.bnd_reg, bound)
            self.bnd_val = bound
        assert bound == self.bnd_val
        inst = self.nc.gpsimd.indirect_dma_start(out=out, out_offset=out_off, in_=in_, in_offset=in_off,
                                                 bounds_check=self.bnd_reg, oob_is_err=False)
        ent[1] += 16
        inst.then_inc(ent[0], 16)
        tok = ("d:%d" % ent[2], ent[0], ent[1])
        self._update(tok, r, w)
        return inst

    def barrier(self):
        for e in self.E:
            for e2 in self.E:
                if e2 != e and self.ecnt[e2] > 0:
                    self._wait(e, ("e:" + e2, self.esem[e2], self.ecnt[e2]))
            for key, ent in self.dsem.items():
                if ent[1] > 0:
                    self._wait(e, ("d:%d" % ent[2], ent[0], ent[1]))
        self.pool.extend(self.dsem.values())
        self.dsem = {}
        self.lastw = {}
        self.readers = {}

    def ps(self):
        ring = self.ps_ring
        i = ring[self.nps % len(ring)]
        self.nps += 1
        return self.ps_tiles[i], "ps%d" % i

    def ps_fixed(self, i):
        return self.ps_tiles[i], "ps%d" % i

    def pb(self):
        i = self.npb % len(self.pb_tiles)
        self.npb += 1
        return self.pb_tiles[i], "pb%d" % i


def build_program(dbg=None, stop_after=99, step=99):
    nc = bass.Bass("TRN2", target_bir_lowering=False)
    dbg = dbg or []
    din = {}

    def inp(name, shape, dt=F32):
        din[name] = nc.dram_tensor(name, list(shape), dt, kind="ExternalInput").ap()
        return din[name]

    def scratch(name, shape, dt):
        kind = "ExternalOutput" if name in dbg else "Internal"
        return nc.dram_tensor(name, list(shape), dt, kind=kind).ap()

    x_ctx = inp("x_ctx", [NTOK, D]); x_own = inp("x_own", [NTOK, D])
    pos_row = inp("pos_row", [1, NEXT], I32); pos_col = inp("pos_col", [128, 16], I32)
    c_col = inp("c_col", [128, 16]); flag = inp("flag", [128, 1])
    ident_in = inp("ident", [128, 128]); invf_in = inp("invf", [64, 1]); iota_in = inp("iota128", [128, 128])
    b_ada = inp("b_ada", [1, 6 * D]); gvec_in = inp("gvec", [128, 64])
    gq_in = inp("gq", [128, 4]); gkv_in = inp("gkv", [128, 4])
    w_ada = inp("w_ada", [D, 6 * D]); w_in = inp("w_in", [D, 6208])
    w_uq = inp("w_uq", [512, 1536]); w_uk = inp("w_uk", [512, 1024]); w_uv = inp("w_uv", [512, 1024])
    a_re_sl = inp("a_re_sl", [128, 32]); a_im_sl = inp("a_im_sl", [128, 32]); ldt2 = inp("ldt2", [2, 32])
    b_re_sl = inp("b_re_sl", [128, 32, 16]); b_im_sl = inp("b_im_sl", [128, 32, 16])
    c_re_sl = inp("c_re_sl", [128, 32, 16]); c_im_sl = inp("c_im_sl", [128, 32, 16])
    dcol_in = inp("dcol", [128, 8]); w_glu = inp("w_glu", [1024, 1024])
    w_br_mla = inp("w_br_mla", [1024, D]); w_br_s5 = inp("w_br_s5", [1024, D]); w_out = inp("w_out", [D, D])
    w_router = inp("w_router", [D, NE]); rbias_in = inp("router_bias", [1, NE]); ecap_in = inp("ecap", [1, NE]); ltri_in = inp("ltri", [128, 128])
    w_eg = inp("w_exp_gate", [NE, D, 512]); w_eu = inp("w_exp_up", [NE, D, 512]); w_ed = inp("w_exp_down", [NE, 512, D])
    w_sg = inp("w_sh_gate", [D, 512]); w_su = inp("w_sh_up", [D, 512]); w_sd = inp("w_sh_down", [512, D])
    out_d = nc.dram_tensor("out", [NTOK, D], F32, kind="ExternalOutput").ap()
    s_idx = scratch("s_idx", [128, 64], I32)
    s_x1 = scratch("s_x1", [NTOK, D], F32); s_h2T = scratch("s_h2T", [128, 16, NTOK], BF16)
    s_xe = scratch("s_xe", [NE * CAP, D], BF16); s_ye = scratch("s_ye", [NE * CAP, D], BF16); s_sh = scratch("s_sh", [NTOK, D], F32)
    s_attn = scratch("s_attn", [128, 8, NTOK], BF16)
    s_s5 = scratch("s_s5", [128, 8, NTOK], BF16)

    s_qn = scratch("s_qn", [128, 4, NTOK], BF16)
    s_ckv = scratch("s_ckv", [128, 4, NEXT], BF16)
    s_kpe = scratch("s_kpe", [64, NEXT], BF16)
    s_u = scratch("s_u", [128, 8, NEXT], BF16)
    s_hT = scratch("s_hT", [128, 16, NTOK], BF16)
    s_mod = scratch("s_mod", [128, 96], F32)

    w_ada_v = w_ada.rearrange("(k p) n -> p k n", p=128)
    w_in_v = w_in.rearrange("(k p) n -> p k n", p=128)

    with ExitStack() as es:
        kb = KB(nc, es)
        op, dma = kb.op, kb.dma

        def sb(name, shape, dt=F32, st=None):
            return (st or es).enter_context(nc.sbuf_tensor("t_" + name, list(shape), dt))

        kb.ps_tiles = [es.enter_context(nc.psum_tensor("ps%d" % i, [128, 512], F32)) for i in range(6)]
        kb.pb_tiles = [es.enter_context(nc.psum_tensor("pb%d" % i, [128, 1024], BF16)) for i in range(2)]

        ident_f = sb("ident_f", [128, 128]); ident_b = sb("ident_b", [128, 128], BF16)
        ones_f = sb("ones_f", [128, 128]); ones_b = sb("ones_b", [128, 128], BF16)
        one11 = sb("one11", [1, 1])
        modT = sb("modT", [128, 96])
        vecs = sb("vecs", [128, 6, 16])
        gv = sb("gv", [128, 64])
        flag_t = sb("flag_t", [128, 1])
        sbc = sb("sbc", [128, D]); shbc = sb("shbc", [128, D])
        epsc = sb("epsc", [128, 1])

        dma("sp", ident_f[:], ident_in, w=["ident_f"])
        dma("sp", gv[:], gvec_in, w=["gv"])
        dma("sp", flag_t[:], flag, w=["flag_t"])
        op("dve", lambda e: e.tensor_copy(out=ident_b[:], in_=ident_f[:]), r=["ident_f"], w=["ident_b"])
        op("dve", lambda e: e.memset(ones_f[:], 1.0), w=["ones_f"])
        op("dve", lambda e: e.memset(ones_b[:], 1.0), w=["ones_b"])
        op("dve", lambda e: e.memset(one11[:], 1.0), w=["one11"])
        op("dve", lambda e: e.memset(epsc[:], EPS), w=["epsc"])

        scb = sb("scb", [128, 16], BF16)

        def mod_block(nb, sl, slk, br, brk, rt, rk):
            dma("pool", sl[:], w_ada_v[:, :, nb * 512:(nb + 1) * 512], w=[slk])
            dma("sp", br[:], b_ada[0:1, nb * 512:(nb + 1) * 512], w=[brk])

            def compute():
                pt, pk = kb.ps()
                for k in range(16):
                    op("pe", lambda e, k=k: e.matmul(pt[0:1, :], scb[:, k:k + 1], sl[:, k, :], start=(k == 0), stop=(k == 15)),
                       r=["scb", slk], w=[pk], inc=(k == 15))
                op("dve", lambda e: e.tensor_tensor(out=rt[:], in0=pt[0:1, :], in1=br[:], op=ALU.add), r=[pk, brk], w=[rk])
                pc, pck = kb.ps()
                for j in range(4):
                    op("pe", lambda e, j=j: e.matmul(pc[:, j:j + 1], rt[0:1, j * 128:(j + 1) * 128], one11[0:1, 0:1], start=True, stop=True),
                       r=[rk, "one11"], w=[pck], inc=(j == 3))
                op("dve", lambda e: e.tensor_copy(out=modT[:, nb * 4:(nb + 1) * 4], in_=pc[:, 0:4]), r=[pck], w=["modT"])
            return compute

        def vec_s(dst, sc0, g0):
            op("dve", lambda e: e.scalar_tensor_tensor(out=vecs[:, dst, :], in0=modT[:, sc0:sc0 + 16], scalar=1.0,
                                                       in1=gv[:, g0:g0 + 16], op0=ALU.add, op1=ALU.mult),
               r=["modT", "gv"], w=["vecs"])

        with ExitStack() as p0:
            ccol = sb("ccol", [128, 16], F32, p0)
            ring = [sb("p0ring%d" % i, [128, 16, 512], BF16, p0) for i in range(2)]
            brow = [sb("brow%d" % i, [1, 512], F32, p0) for i in range(2)]
            rowt = [sb("rowt%d" % i, [1, 512], F32, p0) for i in range(2)]
            dma("sp", ccol[:], c_col, w=["ccol"])
            op("act", lambda e: e.activation(out=scb[:], in_=ccol[:], func=AF.Silu), r=["ccol"], w=["scb"])
            for nb in range(8):
                i = nb % 2
                mod_block(nb, ring[i], "p0ring%d" % i, brow[i], "brow%d" % i, rowt[i], "rowt%d" % i)()
            vec_s(0, 16, 0)
            op("dve", lambda e: e.tensor_copy(out=vecs[:, 1, :], in_=modT[:, 0:16]), r=["modT"], w=["vecs"])
            kb.barrier()

        def mod_rest_vectors():
            op("dve", lambda e: e.tensor_tensor(out=vecs[:, 2, :], in0=modT[:, 32:48], in1=gv[:, 16:32], op=ALU.mult), r=["modT", "gv"], w=["vecs"])
            vec_s(3, 64, 32)
            op("dve", lambda e: e.tensor_copy(out=vecs[:, 4, :], in_=modT[:, 48:64]), r=["modT"], w=["vecs"])
            op("dve", lambda e: e.tensor_tensor(out=vecs[:, 5, :], in0=modT[:, 80:96], in1=gv[:, 48:64], op=ALU.mult), r=["modT", "gv"], w=["vecs"])
            if "s_mod" in dbg:
                dma("sp", s_mod, modT[:], r=["modT"], key="dbg")

        bcn = [0]

        def build_bc(dst, dstk, vi, st):
            bcn[0] += 1
            diag = [sb("diag%d_%s_%d" % (i, dstk, bcn[0]), [128, 128], F32, st) for i in range(2)]
            for q4 in range(4):
                pt, pk = kb.ps()
                for j in range(4):
                    fc = q4 * 4 + j
                    dg = diag[fc % 2]; dk = "diag%d_%s_%d" % (fc % 2, dstk, bcn[0])
                    op("dve", lambda e, fc=fc, dg=dg: e.tensor_scalar(out=dg[:], in0=ident_f[:], scalar1=vecs[:, vi, fc:fc + 1], scalar2=None, op0=ALU.mult),
                       r=["ident_f", "vecs"], w=[dk])
                    op("pe", lambda e, j=j, dg=dg: e.matmul(pt[:, j * 128:(j + 1) * 128], ones_f[:], dg[:], start=True, stop=True),
                       r=["ones_f", dk], w=[pk])
                op("act", lambda e, q4=q4: e.copy(out=dst[:, q4 * 512:(q4 + 1) * 512], in_=pt[:]), r=[pk], w=[dstk])

        def prenorm_block(st_tiles, x_src, x_is_dram, tb, hT, hTk, names=("junk", "ss", "rstd", "pn_tmp")):
            xt, xtk, junk, ss, rstd, tmp, htok, htk = st_tiles
            kj, kss, krs, ktm = names
            if x_is_dram:
                dma("sp", xt[:], x_src, w=[xtk])
            op("act", lambda e: e.activation(out=junk[:], in_=xt[:], func=AF.Square, accum_out=ss[:]), r=[xtk], w=[kj, kss])
            op("act", lambda e: e.activation(out=rstd[:], in_=ss[:], func=AF.Sqrt, bias=epsc[:], scale=1.0 / D), r=[kss, "epsc"], w=[krs])
            op("dve", lambda e: e.reciprocal(out=rstd[:], in_=rstd[:]), r=[krs], w=[krs])
            op("dve", lambda e: e.tensor_tensor(out=tmp[:], in0=xt[:], in1=sbc[:], op=ALU.mult), r=[xtk, "sbc"], w=[ktm])
            op("dve", lambda e: e.scalar_tensor_tensor(out=htok[:], in0=tmp[:], scalar=rstd[:, 0:1], in1=shbc[:], op0=ALU.mult, op1=ALU.add),
               r=[ktm, krs, "shbc"], w=[htk])
            for half in range(2):
                pt, pk = kb.pb()
                for j in range(8):
                    fc = half * 8 + j
                    op("pe", lambda e, j=j, fc=fc: e.transpose(pt[:, j * 128:(j + 1) * 128], htok[:, fc * 128:(fc + 1) * 128], ident_b[:]),
                       r=[htk, "ident_b"], w=[pk], inc=(j == 7))
                eng = "act" if half == 0 else "dve"
                if eng == "act":
                    op("act", lambda e: e.copy(out=hT[:, half * 8:(half + 1) * 8, tb * 128:(tb + 1) * 128],
                                               in_=pt[:].rearrange("p (c t) -> p c t", c=8)), r=[pk], w=[hTk])
                else:
                    op("dve", lambda e: e.tensor_copy(out=hT[:, half * 8:(half + 1) * 8, tb * 128:(tb + 1) * 128],
                                                      in_=pt[:].rearrange("p (c t) -> p c t", c=8)), r=[pk], w=[hTk])

        if stop_after >= 1:
            with ExitStack() as p1:
                hT = sb("hT", [128, 16, NTOK], BF16, p1)
                build_bc(sbc, "sbc", 0, p1)
                build_bc(shbc, "shbc", 1, p1)
                xts = [sb("xt%d" % i, [128, D], F32, p1) for i in range(2)]
                junk = sb("junk", [128, D], BF16, p1); ss = sb("ss", [128, 1], F32, p1); rstd = sb("rstd", [128, 1], F32, p1)
                tmp = sb("pn_tmp", [128, D], F32, p1)
                htoks = [sb("htok%d" % i, [128, D], BF16, p1) for i in range(2)]
                ring = [sb("p1ring%d" % i, [128, 16, 512], BF16, p1) for i in range(2)]
                nring = [0]
                wkpe = sb("wkpe", [128, 16, 64], BF16, p1); wrot = sb("wrot", [128, 16, 64], BF16, p1)
                gq = sb("gq", [128, 4], F32, p1); gkv = sb("gkv", [128, 4], F32, p1)
                sqs = [sb("sq%d" % i, [128, 512], BF16, p1) for i in range(4)]
                rbc = sb("rbc", [128, 512], F32, p1)
                lat_o = sb("lat_o", [128, 4, NTOK], BF16, p1); lat_q = sb("lat_q", [128, 4, NTOK], BF16, p1)
                u_o = sb("u_o", [128, 8, NTOK], BF16, p1)
                kpe_o = sb("kpe_o", [64, NTOK], BF16, p1)
                posb = sb("posb", [64, NTOK], I32, p1); ang = sb("ang", [64, NTOK], F32, p1); angi = sb("angi", [64, NTOK], I32, p1)
                cosTs = [sb("cosT%d" % i, [64, NTOK], F32, p1) for i in range(2)]; sinTs = [sb("sinT%d" % i, [64, NTOK], F32, p1) for i in range(2)]
                invf = sb("invf", [64, 1], F32, p1)
                kt1 = sb("kt1", [64, 512], F32, p1); kt2 = sb("kt2", [64, 512], F32, p1)
                dma("sp", gq[:], gq_in, w=["gq"]); dma("sp", gkv[:], gkv_in, w=["gkv"]); dma("sp", invf[:], invf_in, w=["invf"])
                dma("pool", wkpe[:], w_in_v[:, :, 1024:1088], w=["wkpe"])
                op("dve", lambda e: e.tensor_scalar(out=wrot[:, :, 0:32], in0=wkpe[:, :, 32:64], scalar1=-1.0, scalar2=None, op0=ALU.mult), r=["wkpe"], w=["wrot"])
                op("dve", lambda e: e.tensor_copy(out=wrot[:, :, 32:64], in_=wkpe[:, :, 0:32]), r=["wkpe"], w=["wrot"])

                def rope_tables(tok0):
                    pi_ = 0 if tok0 == 0 else 1
                    dma("sp", posb[:], pos_row[0:1, tok0:tok0 + NTOK].partition_broadcast(64), w=["posb"])
                    for (dst, dk, off) in ((sinTs[pi_], "sinT%d" % pi_, 0.0), (cosTs[pi_], "cosT%d" % pi_, 0.25)):
                        op("dve", lambda e, off=off: e.tensor_scalar(out=ang[:], in0=posb[:], scalar1=invf[:, 0:1], scalar2=off, op0=ALU.mult, op1=ALU.add),
                           r=["posb", "invf"], w=["ang"])
                        op("dve", lambda e: e.tensor_copy(out=angi[:], in_=ang[:]), r=["ang"], w=["angi"])
                        op("dve", lambda e: e.tensor_tensor(out=ang[:], in0=ang[:], in1=angi[:], op=ALU.subtract), r=["ang", "angi"], w=["ang"])
                        op("act", lambda e, dst=dst: e.activation(out=dst[:], in_=ang[:], func=AF.Sin, scale=TWO_PI * (1 - 1e-6)), r=["ang"], w=[dk])

                def load_w(c0, ncols=512):
                    i = nring[0] % 2; nring[0] += 1
                    dma("pool", ring[i][:, :, 0:ncols], w_in_v[:, :, c0:c0 + ncols], w=["p1ring%d" % i])
                    return ring[i], "p1ring%d" % i

                def mm_group(pt, pk, wt, wk, c0, m, tt):
                    for k in range(16):
                        op("pe", lambda e, k=k: e.matmul(pt[0:m, :], wt[:, k, c0:c0 + m], hT[:, k, tt * 512:(tt + 1) * 512], start=(k == 0), stop=(k == 15)),
                           r=[wk, "hT%d" % tt], w=[pk], inc=(k == 15))

                def seg_lat(c0, g, gk, tt, lo, lok):
                    wt, wk = load_w(c0)
                    pts = []
                    for c in range(4):
                        pt, pk = kb.ps(); pts.append((pt, pk))
                        mm_group(pt, pk, wt, wk, c * 128, 128, tt)
                        op("act", lambda e, c=c, pt=pt: e.activation(out=sqs[c][:], in_=pt[:], func=AF.Square), r=[pk], w=["sq%d" % c])
                    pss, pssk = kb.ps()
                    for c in range(4):
                        op("pe", lambda e, c=c: e.matmul(pss[:], ones_b[:], sqs[c][:], start=(c == 0), stop=(c == 3)),
                           r=["ones_b", "sq%d" % c], w=[pssk], inc=(c == 3))
                    op("act", lambda e: e.activation(out=rbc[:], in_=pss[:], func=AF.Sqrt, bias=epsc[:], scale=1.0 / 512), r=[pssk, "epsc"], w=["rbc"])
                    op("dve", lambda e: e.reciprocal(out=rbc[:], in_=rbc[:]), r=["rbc"], w=["rbc"])
                    for c in range(4):
                        pt, pk = pts[c]
                        op("dve", lambda e, c=c, pt=pt: e.scalar_tensor_tensor(out=lo[:, c, tt * 512:(tt + 1) * 512], in0=pt[:], scalar=g[:, c:c + 1],
                                                                               in1=rbc[:], op0=ALU.mult, op1=ALU.mult),
                           r=[pk, gk, "rbc"], w=[lok])

                def seg_kpe(tt, pi_):
                    cosT = cosTs[pi_]; sinT = sinTs[pi_]
                    pa, pak = kb.ps(); pbt, pbk = kb.ps()
                    mm_group(pa, pak, wkpe, "wkpe", 0, 64, tt)
                    mm_group(pbt, pbk, wrot, "wrot", 0, 64, tt)
                    sl = slice(tt * 512, (tt + 1) * 512)
                    op("dve", lambda e: e.tensor_tensor(out=kt1[:], in0=pa[0:64, :], in1=cosT[:, sl], op=ALU.mult), r=[pak, "cosT%d" % pi_], w=["kt1"])
                    op("dve", lambda e: e.tensor_tensor(out=kt2[:], in0=pbt[0:64, :], in1=sinT[:, sl], op=ALU.mult), r=[pbk, "sinT%d" % pi_], w=["kt2"])
                    op("dve", lambda e: e.tensor_tensor(out=kpe_o[:, sl], in0=kt1[:], in1=kt2[:], op=ALU.add), r=["kt1", "kt2"], w=["kpe_o"])

                def seg_u(tt, use_flag):
                    for hb in range(2):
                        wt, wk = load_w(1088 + hb * 512)
                        for c in range(4):
                            pt, pk = kb.ps()
                            mm_group(pt, pk, wt, wk, c * 128, 128, tt)
                            if use_flag:
                                op("dve", lambda e, pt=pt, c=c: e.tensor_scalar(out=u_o[:, hb * 4 + c, tt * 512:(tt + 1) * 512], in0=pt[:], scalar1=flag_t[:, 0:1], scalar2=None, op0=ALU.mult),
                                   r=[pk, "flag_t"], w=["u_o"])
                            else:
                                op("act", lambda e, pt=pt, c=c: e.copy(out=u_o[:, hb * 4 + c, tt * 512:(tt + 1) * 512], in_=pt[:]), r=[pk], w=["u_o"])

                passes = ((x_ctx, 0), (x_own, NTOK))

                def PN(ps_i, tt):
                    xsrc, tok0 = passes[ps_i]
                    if tt == 0:
                        rope_tables(tok0)
                    for tb in range(tt * 4, tt * 4 + 4):
                        i = tb % 2
                        prenorm_block((xts[i], "xt%d" % i, junk, ss, rstd, tmp, htoks[i], "htok%d" % i), xsrc[tb * 128:(tb + 1) * 128, :], True, tb, hT, "hT%d" % tt)

                def MM(ps_i, tt):
                    xsrc, tok0 = passes[ps_i]
                    if ps_i == 1:
                        seg_lat(0, gq, "gq", tt, lat_q, "lat_q")
                        if tt == 1:
                            dma("sp", s_qn, lat_q[:], r=["lat_q"], key="st_latq")
                    seg_lat(512, gkv, "gkv", tt, lat_o, "lat_o")
                    if tt == 1:
                        dma("sp", s_ckv[:, :, tok0:tok0 + NTOK], lat_o[:], r=["lat_o"], key="st_lat")
                    seg_kpe(tt, ps_i)
                    seg_u(tt, ps_i == 0)
                    if tt == 1:
                        dma("sp", s_kpe[:, tok0:tok0 + NTOK], kpe_o[:], r=["kpe_o"], key="st_kpe")
                        dma("sp", s_u[:, :, tok0:tok0 + NTOK], u_o[:], r=["u_o"], key="st_u")

                PN(0, 0); PN(0, 1); MM(0, 0); PN(1, 0); MM(0, 1); PN(1, 1); MM(1, 0); MM(1, 1)
                dma("sp", s_hT, hT[:], r=["hT0", "hT1"], key="st_hT")
                kb.barrier()


        if stop_after >= 2:
            with ExitStack() as p2:
                qn = sb("qn", [128, 4, NTOK], BF16, p2); ckv = sb("ckv", [128, 4, NEXT], BF16, p2); kpe = sb("kpe", [64, NEXT], BF16, p2)
                wuq = sb("wuq", [128, 4, 1536], BF16, p2); wuk = sb("wuk", [128, 4, 1024], BF16, p2); wuv = sb("wuv", [128, 4, 1024], BF16, p2)
                wqr = sb("wqr", [128, 4, 8, 64], BF16, p2)
                dma("sp", qn[:], s_qn, w=["qn"]); dma("sp", ckv[:], s_ckv, w=["ckv"]); dma("sp", kpe[:], s_kpe, w=["kpe"])
                dma("pool", wuq[:], w_uq.rearrange("(k p) n -> p k n", p=128), w=["wuq"])
                dma("pool", wuk[:], w_uk.rearrange("(k p) n -> p k n", p=128), w=["wuk"])
                dma("pool", wuv[:], w_uv.rearrange("(k p) n -> p k n", p=128), w=["wuv"])
                wuq4 = wuq[:].rearrange("p k (h d) -> p k h d", h=8)
                for k in range(4):
                    op("dve", lambda e, k=k: e.tensor_scalar(out=wqr[:, k, :, 0:32], in0=wuq4[:, k, :, 160:192], scalar1=-1.0, scalar2=None, op0=ALU.mult), r=["wuq"], w=["wqr"])
                    op("dve", lambda e, k=k: e.tensor_copy(out=wqr[:, k, :, 32:64], in_=wuq4[:, k, :, 128:160]), r=["wuq"], w=["wqr"])
                posb = sb("posb2", [64, NTOK], I32, p2); ang = sb("ang2", [64, NTOK], F32, p2); angi = sb("angi2", [64, NTOK], I32, p2)
                cosT = sb("cosT2", [64, NTOK], F32, p2); sinT = sb("sinT2", [64, NTOK], F32, p2); invf = sb("invf2", [64, 1], F32, p2)
                dma("sp", invf[:], invf_in, w=["invf2"])
                dma("sp", posb[:], pos_row[0:1, NTOK:NEXT].partition_broadcast(64), w=["posb2"])
                for (dst, dk, off) in ((sinT, "sinT2", 0.0), (cosT, "cosT2", 0.25)):
                    op("dve", lambda e, off=off: e.tensor_scalar(out=ang[:], in0=posb[:], scalar1=invf[:, 0:1], scalar2=off, op0=ALU.mult, op1=ALU.add), r=["posb2", "invf2"], w=["ang2"])
                    op("dve", lambda e: e.tensor_copy(out=angi[:], in_=ang[:]), r=["ang2"], w=["angi2"])
                    op("dve", lambda e: e.tensor_tensor(out=ang[:], in0=ang[:], in1=angi[:], op=ALU.subtract), r=["ang2", "angi2"], w=["ang2"])
                    op("act", lambda e, dst=dst: e.activation(out=dst[:], in_=ang[:], func=AF.Sin, scale=TWO_PI * (1 - 1e-6)), r=["ang2"], w=[dk])
                pqi = sb("pqi", [128, NTOK], I32, p2); cq = sb("cq", [128, NTOK], F32, p2)
                pki = sb("pki", [128, 16], I32, p2); ck = sb("ck", [128, 16], F32, p2)
                cb = sb("cb", [128, 8], F32, p2); msk = sb("msk", [128, 8, 128], BF16, p2)
                dma("sp", pqi[:], pos_row[0:1, NTOK:NEXT].partition_broadcast(128), w=["pqi"])
                dma("sp", pki[:], pos_col, w=["pki"])
                op("dve", lambda e: e.tensor_single_scalar(out=pqi[:], in_=pqi[:], scalar=6, op=ALU.arith_shift_right), r=["pqi"], w=["pqi"])
                op("dve", lambda e: e.tensor_copy(out=cq[:], in_=pqi[:]), r=["pqi"], w=["cq"])
                op("dve", lambda e: e.tensor_single_scalar(out=pki[:], in_=pki[:], scalar=6, op=ALU.arith_shift_right), r=["pki"], w=["pki"])
                op("dve", lambda e: e.tensor_copy(out=ck[:], in_=pki[:]), r=["pki"], w=["ck"])
                op("dve", lambda e: e.tensor_scalar(out=cb[:], in0=ck[:, 0:8], scalar1=cq[:, 0:1], scalar2=-30000.0, op0=ALU.is_gt, op1=ALU.mult), r=["ck", "cq"], w=["cb"])
                for j in range(8):
                    op("dve", lambda e, j=j: e.tensor_scalar(out=msk[:, j, :], in0=cq[:, j * 128:(j + 1) * 128], scalar1=ck[:, 8 + j:9 + j], scalar2=None, op0=ALU.is_ge), r=["cq", "ck"], w=["msk"])
                attnT = sb("attnT", [128, 8, NTOK], BF16, p2)
                qhn = [sb("qhn%d" % i, [128, NTOK], BF16, p2) for i in range(2)]
                qhp = [sb("qhp%d" % i, [64, NTOK], BF16, p2) for i in range(2)]
                khs = [sb("kh%d" % i, [128, NEXT], BF16, p2) for i in range(2)]
                vhs = [sb("vh%d" % i, [128, 16, 128], BF16, p2) for i in range(2)]
                pTs = [sb("pT%d" % i, [128, 512], BF16, p2) for i in range(4)]
                qt1 = sb("qt1", [64, 512], F32, p2); qt2 = sb("qt2", [64, 512], F32, p2)
                rden = sb("rden", [128, 512], F32, p2)
                npT = [0]
                SC = float(192 ** -0.5)
                mring = [sb("p2ring%d" % i, [128, 16, 512], BF16, p2) for i in range(2)]
                mbrow = [sb("mbrow%d" % i, [1, 512], F32, p2) for i in range(2)]
                mrowt = [sb("mrowt%d" % i, [1, 512], F32, p2) for i in range(2)]
                for h in range(8):
                    b = h % 2
                    mod_pending = [mod_block(8 + 2 * h + i, mring[i], "p2ring%d" % i, mbrow[i], "mbrow%d" % i, mrowt[i], "mrowt%d" % i) for i in range(2)]
                    qn_h, qp_h, kh, vh = qhn[b], qhp[b], khs[b], vhs[b]
                    kq, kp, kk, kv = "qhn%d" % b, "qhp%d" % b, "kh%d" % b, "vh%d" % b
                    for tt in range(2):
                        sl = slice(tt * 512, (tt + 1) * 512)
                        pt, pk = kb.ps()
                        for k in range(4):
                            op("pe", lambda e, k=k: e.matmul(pt[:], wuq[:, k, 192 * h:192 * h + 128], qn[:, k, sl], start=(k == 0), stop=(k == 3)), r=["wuq", "qn"], w=[pk], inc=(k == 3))
                        op("act", lambda e: e.copy(out=qn_h[:, sl], in_=pt[:]), r=[pk], w=[kq])
                        pa, pak = kb.ps(); pbt, pbk = kb.ps()
                        for k in range(4):
                            op("pe", lambda e, k=k: e.matmul(pa[0:64, :], wuq[:, k, 192 * h + 128:192 * h + 192], qn[:, k, sl], start=(k == 0), stop=(k == 3)), r=["wuq", "qn"], w=[pak], inc=(k == 3))
                        for k in range(4):
                            op("pe", lambda e, k=k: e.matmul(pbt[0:64, :], wqr[:, k, h, :], qn[:, k, sl], start=(k == 0), stop=(k == 3)), r=["wqr", "qn"], w=[pbk], inc=(k == 3))
                        op("dve", lambda e: e.tensor_tensor(out=qt1[:], in0=pa[0:64, :], in1=cosT[:, sl], op=ALU.mult), r=[pak, "cosT2"], w=["qt1"])
                        op("dve", lambda e: e.tensor_tensor(out=qt2[:], in0=pbt[0:64, :], in1=sinT[:, sl], op=ALU.mult), r=[pbk, "sinT2"], w=["qt2"])
                        op("dve", lambda e: e.tensor_tensor(out=qp_h[:, sl], in0=qt1[:], in1=qt2[:], op=ALU.add), r=["qt1", "qt2"], w=[kp])
                    for t4 in range(4):
                        sl = slice(t4 * 512, (t4 + 1) * 512)
                        pt, pk = kb.ps()
                        for k in range(4):
                            op("pe", lambda e, k=k: e.matmul(pt[:], wuk[:, k, 128 * h:128 * h + 128], ckv[:, k, sl], start=(k == 0), stop=(k == 3)), r=["wuk", "ckv"], w=[pk], inc=(k == 3))
                        op("act", lambda e: e.copy(out=kh[:, sl], in_=pt[:]), r=[pk], w=[kk])
                    for t4 in range(4):
                        pt, pk = kb.ps()
                        for j in range(4):
                            tb = t4 * 4 + j
                            for k in range(4):
                                op("pe", lambda e, k=k, tb=tb, j=j: e.matmul(pt[:, j * 128:(j + 1) * 128], ckv[:, k, tb * 128:(tb + 1) * 128], wuv[:, k, 128 * h:128 * h + 128],
                                                                        start=(k == 0), stop=(k == 3)), r=["wuv", "ckv"], w=[pk], inc=(k == 3 and j == 3))
                        op("dve", lambda e: e.tensor_copy(out=vh[:, t4 * 4:(t4 + 1) * 4, :], in_=pt[:].rearrange("p (j d) -> p j d", j=4)), r=[pk], w=[kv])
                    kb.ps_ring = [0, 1, 2, 3]
                    for qt in range(2):
                        po, pok = kb.ps_fixed(4); pd, pdk = kb.ps_fixed(5)
                        blocks = [(jb, 0, True) for jb in range(8)] + [(8 + j, max(0, j - 4 * qt) * 128, False) for j in range(4 * qt + 4)]
                        def emit_S(bi):
                            eb, c0, is_ctx = blocks[bi]
                            pss, pssk = kb.ps()
                            q0 = qt * 512 + c0
                            op("pe", lambda e: e.matmul(pss[:, c0:512], kh[:, eb * 128:(eb + 1) * 128], qn_h[:, q0:qt * 512 + 512], start=True, stop=False), r=[kk, kq], w=[pssk], inc=False)
                            op("pe", lambda e: e.matmul(pss[:, c0:512], kpe[:, eb * 128:(eb + 1) * 128], qp_h[:, q0:qt * 512 + 512], start=False, stop=True), r=["kpe", kp], w=[pssk])
                            pi = npT[0] % 4; npT[0] += 1
                            pT = pTs[pi]; pTk = "pT%d" % pi
                            if is_ctx:
                                op("act", lambda e: e.activation(out=pT[:, c0:512], in_=pss[:, c0:512], func=AF.Exp, bias=cb[:, eb:eb + 1], scale=SC), r=[pssk, "cb"], w=[pTk])
                            else:
                                op("act", lambda e: e.activation(out=pT[:, c0:512], in_=pss[:, c0:512], func=AF.Exp, scale=SC), r=[pssk], w=[pTk])
                                j = eb - 8
                                if j >= 4 * qt:
                                    op("pool", lambda e: e.tensor_tensor(out=pT[:, c0:c0 + 128], in0=pT[:, c0:c0 + 128], in1=msk[:, j, :], op=ALU.mult), r=[pTk, "msk"], w=[pTk])
                            return pT, pTk

                        def emit_PV(bi, pT, pTk):
                            eb, c0, is_ctx = blocks[bi]
                            last = (bi == len(blocks) - 1)
                            op("pe", lambda e: e.matmul(po[:, c0:512], vh[:, eb, :], pT[:, c0:512], start=(bi == 0), stop=last), r=[kv, pTk], w=[pok], inc=False)
                            op("pe", lambda e: e.matmul(pd[:, c0:512], ones_b[:], pT[:, c0:512], start=(bi == 0), stop=last), r=["ones_b", pTk], w=[pdk])

                        pend = [emit_S(0), emit_S(1)]
                        for bi in range(len(blocks)):
                            if bi + 2 < len(blocks):
                                pend.append(emit_S(bi + 2))
                            pT, pTk = pend.pop(0)
                            emit_PV(bi, pT, pTk)
                        op("dve", lambda e: e.reciprocal(out=rden[:], in_=pd[:]), r=[pdk], w=["rden"])
                        op("dve", lambda e: e.tensor_tensor(out=attnT[:, h, qt * 512:(qt + 1) * 512], in0=po[:], in1=rden[:], op=ALU.mult), r=[pok, pdk, "rden"], w=["attnT"])
                    kb.ps_ring = [0, 1, 2, 3, 4, 5]
                    for f_ in mod_pending:
                        f_()
                mod_rest_vectors()
                dma("sp", s_attn, attnT[:], r=["attnT"], key="st_attn")
                kb.barrier()


        if stop_after >= 3:
            with ExitStack() as p3:
                yT = sb("yT", [128, 8, NTOK], BF16, p3)
                with ExitStack() as p3a:
                    def small(name):
                        return sb(name, [128, 32], F32, p3a)
                    are = small("are"); aim = small("aim"); ldt = small("ldt"); stp = small("stp"); mag = small("mag"); th = small("th")
                    th2 = small("th2"); fr = small("fr"); fri = sb("fri", [128, 32], I32, p3a); cs = small("cs"); sn = small("sn")
                    abr = small("abr"); abi = small("abi"); den = small("den"); t_a = small("t_a"); t_b = small("t_b")
                    fre = small("fre"); fim = small("fim"); nfim = small("nfim"); e_c = small("e_c"); e_s = small("e_s")
                    ini_r = small("ini_r"); ini_i = small("ini_i"); c_r = small("c_r"); c_i = small("c_i"); l_a = small("l_a"); l_b = small("l_b")
                    bsr = sb("bsr", [128, 32, 16], F32, p3a); bsi = sb("bsi", [128, 32, 16], F32, p3a)
                    csr = sb("csr", [128, 32, 16], F32, p3a); csi = sb("csi", [128, 32, 16], F32, p3a)
                    bbr = sb("bbr", [128, 32, 16], F32, p3a); bbi = sb("bbi", [128, 32, 16], F32, p3a); tb16 = sb("tb16", [128, 16], F32, p3a)
                    dcol = sb("dcol", [128, 8], F32, p3a); iot = sb("iot", [128, 128], F32, p3a); m0 = sb("m0", [128, 128], F32, p3a)
                    dma("sp", are[:], a_re_sl, w=["are"]); dma("sp", aim[:], a_im_sl, w=["aim"])
                    dma("sp", ldt[0:64, :], ldt2[0:1, :].partition_broadcast(64), w=["ldt"]); dma("sp", ldt[64:128, :], ldt2[1:2, :].partition_broadcast(64), w=["ldt"], key="ldt_b")
                    dma("sp", bsr[:], b_re_sl, w=["bsr"]); dma("sp", bsi[:], b_im_sl, w=["bsi"]); dma("sp", csr[:], c_re_sl, w=["csr"]); dma("sp", csi[:], c_im_sl, w=["csi"])
                    dma("sp", dcol[:], dcol_in, w=["dcol"]); dma("sp", iot[:], iota_in, w=["iot"])
                    V = lambda f, r, w: op("dve", f, r=r, w=w)
                    A = lambda f, r, w: op("act", f, r=r, w=w)

                    def sincos(src, srck, off, dst, dstk, tf, tfk, ti, tik):
                        V(lambda e: e.tensor_scalar(out=tf, in0=src, scalar1=off, scalar2=None, op0=ALU.add), [srck], [tfk])
                        V(lambda e: e.tensor_copy(out=ti, in_=tf), [tfk], [tik])
                        V(lambda e: e.tensor_tensor(out=tf, in0=tf, in1=ti, op=ALU.subtract), [tfk, tik], [tfk])
                        A(lambda e: e.activation(out=dst, in_=tf, func=AF.Sin, scale=TWO_PI * (1 - 1e-6)), [tfk], [dstk])

                    A(lambda e: e.activation(out=stp[:], in_=ldt[:], func=AF.Exp), ["ldt"], ["stp"])
                    V(lambda e: e.tensor_tensor(out=t_a[:], in0=are[:], in1=stp[:], op=ALU.mult), ["are", "stp"], ["t_a"])
                    A(lambda e: e.activation(out=mag[:], in_=t_a[:], func=AF.Exp), ["t_a"], ["mag"])
                    V(lambda e: e.tensor_tensor(out=th[:], in0=aim[:], in1=stp[:], op=ALU.mult), ["aim", "stp"], ["th"])
                    V(lambda e: e.tensor_scalar(out=th2[:], in0=th[:], scalar1=1.0 / TWO_PI, scalar2=None, op0=ALU.mult), ["th"], ["th2"])
                    sincos(th2[:], "th2", 0.0, sn[:], "sn", fr[:], "fr", fri[:], "fri")
                    sincos(th2[:], "th2", 0.25, cs[:], "cs", fr[:], "fr", fri[:], "fri")
                    V(lambda e: e.tensor_tensor(out=abr[:], in0=mag[:], in1=cs[:], op=ALU.mult), ["mag", "cs"], ["abr"])
                    V(lambda e: e.tensor_tensor(out=abi[:], in0=mag[:], in1=sn[:], op=ALU.mult), ["mag", "sn"], ["abi"])
                    V(lambda e: e.tensor_tensor(out=den[:], in0=are[:], in1=are[:], op=ALU.mult), ["are"], ["den"])
                    V(lambda e: e.tensor_tensor(out=t_a[:], in0=aim[:], in1=aim[:], op=ALU.mult), ["aim"], ["t_a"])
                    V(lambda e: e.tensor_tensor(out=den[:], in0=den[:], in1=t_a[:], op=ALU.add), ["den", "t_a"], ["den"])
                    V(lambda e: e.reciprocal(out=den[:], in_=den[:]), ["den"], ["den"])
                    V(lambda e: e.tensor_scalar(out=abr[:], in0=abr[:], scalar1=-1.0, scalar2=None, op0=ALU.add), ["abr"], ["abr"])
                    V(lambda e: e.tensor_tensor(out=t_a[:], in0=abr[:], in1=are[:], op=ALU.mult), ["abr", "are"], ["t_a"])
                    V(lambda e: e.tensor_tensor(out=t_b[:], in0=abi[:], in1=aim[:], op=ALU.mult), ["abi", "aim"], ["t_b"])
                    V(lambda e: e.tensor_tensor(out=t_a[:], in0=t_a[:], in1=t_b[:], op=ALU.add), ["t_a", "t_b"], ["t_a"])
                    V(lambda e: e.tensor_tensor(out=fre[:], in0=t_a[:], in1=den[:], op=ALU.mult), ["t_a", "den"], ["fre"])
                    V(lambda e: e.tensor_tensor(out=t_a[:], in0=abi[:], in1=are[:], op=ALU.mult), ["abi", "are"], ["t_a"])
                    V(lambda e: e.tensor_tensor(out=t_b[:], in0=abr[:], in1=aim[:], op=ALU.mult), ["abr", "aim"], ["t_b"])
                    V(lambda e: e.tensor_tensor(out=t_a[:], in0=t_a[:], in1=t_b[:], op=ALU.subtract), ["t_a", "t_b"], ["t_a"])
                    V(lambda e: e.tensor_tensor(out=fim[:], in0=t_a[:], in1=den[:], op=ALU.mult), ["t_a", "den"], ["fim"])
                    V(lambda e: e.tensor_scalar(out=nfim[:], in0=fim[:], scalar1=-1.0, scalar2=None, op0=ALU.mult), ["fim"], ["nfim"])
                    V(lambda e: e.tensor_scalar(out=t_b[:], in0=th2[:], scalar1=128.0, scalar2=None, op0=ALU.mult), ["th2"], ["t_b"])
                    sincos(t_b[:], "t_b", 0.0, e_s[:], "e_s", fr[:], "fr", fri[:], "fri")
                    sincos(t_b[:], "t_b", 0.25, e_c[:], "e_c", fr[:], "fr", fri[:], "fri")
                    for i in range(32):
                        V(lambda e, i=i: e.tensor_scalar(out=tb16[:], in0=bsr[:, i, :], scalar1=fre[:, i:i + 1], scalar2=None, op0=ALU.mult), ["bsr", "fre"], ["tb16"])
                        V(lambda e, i=i: e.scalar_tensor_tensor(out=bbr[:, i, :], in0=bsi[:, i, :], scalar=nfim[:, i:i + 1], in1=tb16[:], op0=ALU.mult, op1=ALU.add), ["bsi", "nfim", "tb16"], ["bbr"])
                        V(lambda e, i=i: e.tensor_scalar(out=tb16[:], in0=bsi[:, i, :], scalar1=fre[:, i:i + 1], scalar2=None, op0=ALU.mult), ["bsi", "fre"], ["tb16"])
                        V(lambda e, i=i: e.scalar_tensor_tensor(out=bbi[:, i, :], in0=bsr[:, i, :], scalar=fim[:, i:i + 1], in1=tb16[:], op0=ALU.mult, op1=ALU.add), ["bsr", "fim", "tb16"], ["bbi"])
                    W1 = sb("W1", [128, 32, 128], BF16, p3a); W2 = sb("W2", [128, 32, 128], BF16, p3a)
                    LBr = sb("LBr", [128, 32, 128], BF16, p3a); LBi = sb("LBi", [128, 32, 128], BF16, p3a)
                    Dg = sb("Dg", [128, 8, 128], BF16, p3a)

                    def place(Wt, Wk, src, srck, neg=False):
                        V(lambda e: e.memset(Wt[:], 0.0), [], [Wk])
                        W4 = Wt[:].rearrange("p (j r) c -> p j r c", r=4)
                        S4 = src[:].rearrange("p (j r) c -> p j r c", r=4)
                        for r in range(4):
                            for gp in range(2):
                                ps_ = slice(64 * gp, 64 * gp + 64)
                                c0 = (2 * r + gp) * 16
                                if neg:
                                    V(lambda e, r=r, ps_=ps_, c0=c0: e.tensor_scalar(out=W4[ps_, :, r, c0:c0 + 16], in0=S4[ps_, :, r, :], scalar1=-1.0, scalar2=None, op0=ALU.mult), [srck], [Wk])
                                else:
                                    V(lambda e, r=r, ps_=ps_, c0=c0: e.tensor_copy(out=W4[ps_, :, r, c0:c0 + 16], in_=S4[ps_, :, r, :]), [srck], [Wk])

                    place(W1, "W1", bbr, "bbr"); place(W2, "W2", bbi, "bbi")
                    for (Wt, Wk, Lt, Lk) in ((W1, "W1", LBr, "LBr"), (W2, "W2", LBi, "LBi")):
                        for i8 in range(4):
                            pt, pk = kb.pb()
                            for q in range(8):
                                i = i8 * 8 + q
                                op("pe", lambda e, i=i, q=q: e.transpose(pt[:, q * 128:(q + 1) * 128], Wt[:, i, :], ident_b[:]), r=[Wk, "ident_b"], w=[pk], inc=(q == 7))
                            V(lambda e, i8=i8: e.tensor_copy(out=Lt[:, i8 * 8:(i8 + 1) * 8, :], in_=pt[:].rearrange("p (q c) -> p q c", q=8)), [pk], [Lk])
                    place(W1, "W1", csr, "csr"); place(W2, "W2", csi, "csi", neg=True)
                    for j in range(8):
                        V(lambda e, j=j: e.tensor_scalar(out=Dg[:, j, :], in0=ident_f[:], scalar1=dcol[:, j:j + 1], scalar2=None, op0=ALU.mult), ["ident_f", "dcol"], ["Dg"])
                    cosT = sb("s5cos", [128, 32, 128], BF16, p3a); sinT = sb("s5sin", [128, 32, 128], BF16, p3a); dtab = sb("dtab", [128, 32, 128], F32, p3a)
                    tf = sb("tf", [128, 128], F32, p3a); tfi = sb("tfi", [128, 128], I32, p3a)
                    V(lambda e: e.tensor_scalar(out=m0[:], in0=iot[:], scalar1=0.5, scalar2=None, op0=ALU.is_gt), ["iot"], ["m0"])
                    for i in range(32):
                        for (dst, dk, off) in ((sinT, "s5sin", 0.0), (cosT, "s5cos", 0.25)):
                            V(lambda e, i=i, off=off: e.tensor_scalar(out=tf[:], in0=iot[:], scalar1=th2[:, i:i + 1], scalar2=off, op0=ALU.mult, op1=ALU.add), ["iot", "th2"], ["tf"])
                            V(lambda e: e.tensor_copy(out=tfi[:], in_=tf[:]), ["tf"], ["tfi"])
                            V(lambda e: e.tensor_tensor(out=tf[:], in0=tf[:], in1=tfi[:], op=ALU.subtract), ["tf", "tfi"], ["tf"])
                            A(lambda e, i=i, dst=dst: e.activation(out=dst[:, i, :], in_=tf[:], func=AF.Sin, scale=TWO_PI * (1 - 1e-6)), ["tf"], [dk])
                        V(lambda e, i=i: e.tensor_scalar(out=dtab[:, i, :], in0=m0[:], scalar1=mag[:, i:i + 1], scalar2=None, op0=ALU.mult), ["m0", "mag"], ["dtab"])
                    zr = sb("zr", [128, 4096], F32, p3a); zi = sb("zi", [128, 4096], F32, p3a)
                    zr3 = zr[:].rearrange("p (i t) -> p i t", t=128); zi3 = zi[:].rearrange("p (i t) -> p i t", t=128)
                    tts = [[sb("s5t%d_%d" % (q, i), [128, 512], BF16, p3a) for i in range(4)] for q in range(2)]
                    bu_r = [sb("bu_r%d" % i, [128, 512], BF16, p3a) for i in range(2)]; bu_i = [sb("bu_i%d" % i, [128, 512], BF16, p3a) for i in range(2)]
                    xr = sb("xr", [128, 32, 128], BF16, p3a); xi = sb("xi", [128, 32, 128], BF16, p3a)
                    xt1 = sb("sxt1", [128, 4096], BF16, p3a); xt2 = sb("sxt2", [128, 4096], BF16, p3a)
                    uch = [sb("uch%d" % i, [128, 8, 128], BF16, p3a) for i in range(2)]
                    cosF = cosT[:].rearrange("p i t -> p (i t)"); sinF = sinT[:].rearrange("p i t -> p (i t)"); dtF = dtab[:].rearrange("p i t -> p (i t)")
                    xrF = xr[:].rearrange("p i t -> p (i t)"); xiF = xi[:].rearrange("p i t -> p (i t)")
                    for ch in range(16):
                        uc = uch[ch % 2]; uk = "uch%d" % (ch % 2)
                        dma("sp", uc[:], s_u[:, :, ch * 128:(ch + 1) * 128], w=[uk])
                        for j in range(8):
                            pre, prek = kb.ps(); pim, pimk = kb.ps()
                            for r in range(4):
                                i = 4 * j + r
                                op("pe", lambda e, i=i, r=r: e.matmul(pre[:, r * 128:(r + 1) * 128], LBr[:, i, :], uc[:, j, :], start=True, stop=True), r=["LBr", uk], w=[prek], inc=(r == 3))
                            for r in range(4):
                                i = 4 * j + r
                                op("pe", lambda e, i=i, r=r: e.matmul(pim[:, r * 128:(r + 1) * 128], LBi[:, i, :], uc[:, j, :], start=True, stop=True), r=["LBi", uk], w=[pimk], inc=(r == 3))
                            sl = slice(j * 512, (j + 1) * 512)
                            br_ = bu_r[j % 2]; bi_ = bu_i[j % 2]; brk = "bu_r%d" % (j % 2); bik = "bu_i%d" % (j % 2)
                            tt_ = tts[j % 2]; tk_ = ["s5t%d_%d" % (j % 2, i) for i in range(4)]
                            A(lambda e: e.copy(out=br_[:], in_=pre[:]), [prek], [brk])
                            A(lambda e: e.copy(out=bi_[:], in_=pim[:]), [pimk], [bik])
                            V(lambda e: e.tensor_tensor(out=tt_[0][:], in0=br_[:], in1=cosF[:, sl], op=ALU.mult), [brk, "s5cos"], [tk_[0]])
                            V(lambda e: e.tensor_tensor(out=tt_[1][:], in0=bi_[:], in1=sinF[:, sl], op=ALU.mult), [bik, "s5sin"], [tk_[1]])
                            V(lambda e: e.tensor_tensor(out=tt_[2][:], in0=bi_[:], in1=cosF[:, sl], op=ALU.mult), [bik, "s5cos"], [tk_[2]])
                            V(lambda e: e.tensor_tensor(out=tt_[3][:], in0=br_[:], in1=sinF[:, sl], op=ALU.mult), [brk, "s5sin"], [tk_[3]])
                            op("pool", lambda e: e.tensor_tensor(out=zr[:, sl], in0=tt_[0][:], in1=tt_[1][:], op=ALU.add), r=[tk_[0], tk_[1]], w=["zr"])
                            op("pool", lambda e: e.tensor_tensor(out=zi[:, sl], in0=tt_[2][:], in1=tt_[3][:], op=ALU.subtract), r=[tk_[2], tk_[3]], w=["zi"])
                        if ch > 0:
                            V(lambda e: e.tensor_tensor(out=c_r[:], in0=mag[:], in1=ini_r[:], op=ALU.mult), ["mag", "ini_r"], ["c_r"])
                            V(lambda e: e.tensor_tensor(out=c_i[:], in0=mag[:], in1=ini_i[:], op=ALU.mult), ["mag", "ini_i"], ["c_i"])
                            V(lambda e: e.tensor_tensor(out=zr3[:, :, 0], in0=zr3[:, :, 0], in1=c_r[:], op=ALU.add), ["zr", "c_r"], ["zr"])
                            V(lambda e: e.tensor_tensor(out=zi3[:, :, 0], in0=zi3[:, :, 0], in1=c_i[:], op=ALU.add), ["zi", "c_i"], ["zi"])
                        V(lambda e: e.tensor_tensor_scan(out=zr[:], data0=dtF, data1=zr[:], initial=0.0, op0=ALU.mult, op1=ALU.add), ["zr", "dtab"], ["zr"])
                        V(lambda e: e.tensor_tensor_scan(out=zi[:], data0=dtF, data1=zi[:], initial=0.0, op0=ALU.mult, op1=ALU.add), ["zi", "dtab"], ["zi"])
                        if ch < 15:
                            V(lambda e: e.tensor_tensor(out=l_a[:], in0=e_c[:], in1=zr3[:, :, 127], op=ALU.mult), ["e_c", "zr"], ["l_a"])
                            V(lambda e: e.tensor_tensor(out=l_b[:], in0=e_s[:], in1=zi3[:, :, 127], op=ALU.mult), ["e_s", "zi"], ["l_b"])
                            V(lambda e: e.tensor_tensor(out=ini_r[:], in0=l_a[:], in1=l_b[:], op=ALU.subtract), ["l_a", "l_b"], ["ini_r"])
                            V(lambda e: e.tensor_tensor(out=l_a[:], in0=e_s[:], in1=zr3[:, :, 127], op=ALU.mult), ["e_s", "zr"], ["l_a"])
                            V(lambda e: e.tensor_tensor(out=l_b[:], in0=e_c[:], in1=zi3[:, :, 127], op=ALU.mult), ["e_c", "zi"], ["l_b"])
                            V(lambda e: e.tensor_tensor(out=ini_i[:], in0=l_a[:], in1=l_b[:], op=ALU.add), ["l_a", "l_b"], ["ini_i"])
                        if ch >= 8:
                            V(lambda e: e.tensor_tensor(out=xt1[:], in0=zr[:], in1=cosF, op=ALU.mult), ["zr", "s5cos"], ["sxt1"])
                            V(lambda e: e.tensor_tensor(out=xt2[:], in0=zi[:], in1=sinF, op=ALU.mult), ["zi", "s5sin"], ["sxt2"])
                            V(lambda e: e.tensor_tensor(out=xrF, in0=xt1[:], in1=xt2[:], op=ALU.subtract), ["sxt1", "sxt2"], ["xr"])
                            V(lambda e: e.tensor_tensor(out=xt1[:], in0=zr[:], in1=sinF, op=ALU.mult), ["zr", "s5sin", "xr"], ["sxt1"])
                            V(lambda e: e.tensor_tensor(out=xt2[:], in0=zi[:], in1=cosF, op=ALU.mult), ["zi", "s5cos", "xr"], ["sxt2"])
                            V(lambda e: e.tensor_tensor(out=xiF, in0=xt1[:], in1=xt2[:], op=ALU.add), ["sxt1", "sxt2"], ["xi"])
                            oc = ch - 8
                            for j4 in range(2):
                                py, pyk = kb.ps()
                                for jj in range(4):
                                    j = j4 * 4 + jj
                                    for r in range(4):
                                        i = 4 * j + r
                                        op("pe", lambda e, i=i, jj=jj, r=r: e.matmul(py[:, jj * 128:(jj + 1) * 128], W1[:, i, :], xr[:, i, :], start=(r == 0), stop=False), r=["W1", "xr"], w=[pyk], inc=False)
                                        op("pe", lambda e, i=i, jj=jj: e.matmul(py[:, jj * 128:(jj + 1) * 128], W2[:, i, :], xi[:, i, :], start=False, stop=False), r=["W2", "xi"], w=[pyk], inc=False)
                                    op("pe", lambda e, j=j, jj=jj: e.matmul(py[:, jj * 128:(jj + 1) * 128], Dg[:, j, :], uc[:, j, :], start=False, stop=True), r=["Dg", uk], w=[pyk], inc=(jj == 3))
                                A(lambda e, j4=j4: e.activation(out=yT[:, j4 * 4:(j4 + 1) * 4, oc * 128:(oc + 1) * 128], in_=py[:].rearrange("p (j t) -> p j t", j=4), func=AF.Gelu), [pyk], ["yT"])
                    kb.barrier()
                with ExitStack() as p3b:
                    wgl = sb("wgl", [128, 8, 1024], BF16, p3b); s5o = sb("s5o", [128, 8, NTOK], BF16, p3b)
                    sg = [sb("sg%d" % i, [128, 512], F32, p3b) for i in range(2)]
                    dma("pool", wgl[:], w_glu.rearrange("(k p) n -> p k n", p=128), w=["wgl"])
                    n = 0
                    for oc in range(8):
                        for tt in range(2):
                            sl = slice(tt * 512, (tt + 1) * 512)
                            pt, pk = kb.ps()
                            for k in range(8):
                                op("pe", lambda e, k=k: e.matmul(pt[:], wgl[:, k, oc * 128:(oc + 1) * 128], yT[:, k, sl], start=(k == 0), stop=(k == 7)), r=["wgl", "yT"], w=[pk], inc=(k == 7))
                            sgt = sg[n % 2]; sgk = "sg%d" % (n % 2); n += 1
                            op("act", lambda e: e.activation(out=sgt[:], in_=pt[:], func=AF.Sigmoid), r=[pk], w=[sgk])
                            op("dve", lambda e: e.tensor_tensor(out=s5o[:, oc, sl], in0=yT[:, oc, sl], in1=sgt[:], op=ALU.mult), r=["yT", sgk], w=["s5o"])
                    dma("sp", s_s5, s5o[:], r=["s5o"], key="st_s5")
                    kb.barrier()


        if stop_after >= 4:
            idx_all = sb("idx_all", [128, 64], I32); wk_all = sb("wk_all", [128, 64], F32)
            with ExitStack() as p4:
                merged = sb("merged", [128, 16, NTOK], BF16, p4)
                with ExitStack() as p4a:
                    hT = sb("m_hT", [128, 16, NTOK], BF16, p4a); at = sb("m_at", [128, 8, NTOK], BF16, p4a); s5 = sb("m_s5", [128, 8, NTOK], BF16, p4a)
                    dma("sp", hT[:], s_hT, w=["m_hT"]); dma("sp", at[:], s_attn, w=["m_at"]); dma("sp", s5[:], s_s5, w=["m_s5"])
                    wbm = [sb("wbm%d" % i, [128, 8, 256], BF16, p4a) for i in range(2)]; wbs = [sb("wbs%d" % i, [128, 8, 256], BF16, p4a) for i in range(2)]
                    wgm = [sb("wgm%d" % i, [128, 16, 256], BF16, p4a) for i in range(2)]; wgs = [sb("wgs%d" % i, [128, 16, 256], BF16, p4a) for i in range(2)]
                    sgm = sb("sgm", [128, 512], F32, p4a); sgs = sb("sgs", [128, 512], F32, p4a); mt1 = sb("mt1", [128, 512], F32, p4a); mt2 = sb("mt2", [128, 512], F32, p4a)
                    wbm_v = w_br_mla.rearrange("(k p) n -> p k n", p=128); wbs_v = w_br_s5.rearrange("(k p) n -> p k n", p=128)
                    for cb in range(8):
                        i = cb % 2; c0 = cb * 256
                        dma("pool", wbm[i][:], wbm_v[:, :, c0:c0 + 256], w=["wbm%d" % i]); dma("pool", wbs[i][:], wbs_v[:, :, c0:c0 + 256], w=["wbs%d" % i])
                        dma("pool", wgm[i][:], w_in_v[:, :, 2112 + c0:2112 + c0 + 256], w=["wgm%d" % i]); dma("pool", wgs[i][:], w_in_v[:, :, 4160 + c0:4160 + c0 + 256], w=["wgs%d" % i])
                        for f in range(2):
                            fc = cb * 2 + f; fs = slice(f * 128, (f + 1) * 128)
                            for tt in range(2):
                                sl = slice(tt * 512, (tt + 1) * 512)
                                p1_, k1 = kb.ps(); p2_, k2 = kb.ps(); p3_, k3 = kb.ps(); p4_, k4 = kb.ps()
                                for k in range(8):
                                    op("pe", lambda e, k=k: e.matmul(p1_[:], wbm[i][:, k, fs], at[:, k, sl], start=(k == 0), stop=(k == 7)), r=["wbm%d" % i, "m_at"], w=[k1], inc=(k == 7))
                                for k in range(8):
                                    op("pe", lambda e, k=k: e.matmul(p2_[:], wbs[i][:, k, fs], s5[:, k, sl], start=(k == 0), stop=(k == 7)), r=["wbs%d" % i, "m_s5"], w=[k2], inc=(k == 7))
                                for k in range(16):
                                    op("pe", lambda e, k=k: e.matmul(p3_[:], wgm[i][:, k, fs], hT[:, k, sl], start=(k == 0), stop=(k == 15)), r=["wgm%d" % i, "m_hT"], w=[k3], inc=(k == 15))
                                for k in range(16):
                                    op("pe", lambda e, k=k: e.matmul(p4_[:], wgs[i][:, k, fs], hT[:, k, sl], start=(k == 0), stop=(k == 15)), r=["wgs%d" % i, "m_hT"], w=[k4], inc=(k == 15))
                                op("act", lambda e: e.activation(out=sgm[:], in_=p3_[:], func=AF.Sigmoid), r=[k3], w=["sgm"])
                                op("act", lambda e: e.activation(out=sgs[:], in_=p4_[:], func=AF.Sigmoid), r=[k4], w=["sgs"])
                                op("dve", lambda e: e.tensor_tensor(out=mt1[:], in0=p1_[:], in1=sgm[:], op=ALU.mult), r=[k1, "sgm"], w=["mt1"])
                                op("dve", lambda e: e.tensor_tensor(out=mt2[:], in0=p2_[:], in1=sgs[:], op=ALU.mult), r=[k2, "sgs"], w=["mt2"])
                                op("pool", lambda e: e.tensor_tensor(out=merged[:, fc, sl], in0=mt1[:], in1=mt2[:], op=ALU.add), r=["mt1", "mt2"], w=["merged"])
                    kb.barrier()
                with ExitStack() as p4b:
                    wo = sb("wo", [128, 16, D], BF16, p4b); wr = sb("wr", [128, 16, NE], BF16, p4b)
                    for q in range(4):
                        dma("pool", wo[:, :, q * 512:(q + 1) * 512], w_out.rearrange("(k p) n -> p k n", p=128)[:, :, q * 512:(q + 1) * 512], w=["wo"], key="wo%d" % q)
                    dma("pool", wr[:], w_router.rearrange("(k p) n -> p k n", p=128), w=["wr"])
                    gbc = sb("gbc", [128, D], F32, p4b)
                    build_bc(gbc, "gbc", 2, p4b); build_bc(sbc, "sbc", 3, p4b); build_bc(shbc, "shbc", 4, p4b)
                    h2Ts = [sb("h2T%d" % i, [128, 16, 128], BF16, p4b) for i in range(2)]
                    xts = [sb("b_xt%d" % i, [128, D], F32, p4b) for i in range(2)]; x1s = [sb("b_x1%d" % i, [128, D], F32, p4b) for i in range(2)]
                    junk = sb("b_junk", [128, D], BF16, p4b); junk2 = sb("b_junk2", [128, D], BF16, p4b); ss = sb("b_ss", [128, 1], F32, p4b); rstd = sb("b_rstd", [128, 1], F32, p4b)
                    ss4 = sb("b_ss4", [128, 4], F32, p4b); rs1 = sb("b_rs1", [128, 1], F32, p4b)
                    tmp = sb("b_tmp", [128, D], F32, p4b); tmp2 = sb("b_tmp2", [128, D], F32, p4b); htoks = [sb("b_htok%d" % i, [128, D], BF16, p4b) for i in range(2)]
                    rb = sb("rbias", [128, NE], F32, p4b); ecap = sb("ecap", [128, NE], F32, p4b); ltri_f = sb("ltri_f", [128, 128], F32, p4b); ltri = sb("ltri", [128, 128], BF16, p4b)
                    maskall = sb("maskall", [128, 8, NE], BF16, p4b)
                    dma("sp", rb[:], rbias_in.partition_broadcast(128), w=["rbias"]); dma("sp", ecap[:], ecap_in.partition_broadcast(128), w=["ecap"]); dma("sp", ltri_f[:], ltri_in, w=["ltri_f"])
                    op("dve", lambda e: e.tensor_copy(out=ltri[:], in_=ltri_f[:]), r=["ltri_f"], w=["ltri"])
                    R = {}
                    for nm, shp in (("sc", [128, NE]), ("sel", [128, NE]), ("selm", [128, NE]), ("m8g", [128, 8, 8]), ("gs", [128, 8]), ("gtop", [128, 8]), ("gmask", [128, 8]),
                                    ("pen", [128, 8]), ("top8", [128, 8]), ("tmask", [128, NE]), ("wraw", [128, NE]), ("wsum", [128, 1]), ("wfull", [128, NE]),
                                    ("ridx", [128, NE]), ("okm", [128, NE]), ("okt", [128, NE]), ("oh", [128, NE]), ("ot", [128, NE]), ("idxf", [128, 8])):
                        R[nm] = sb("r_" + nm, shp, F32, p4b)
                    V = lambda f, r, w: op("dve", f, r=r, w=w)
                    def stage_A(tb):
                        i = tb % 2
                        xt = xts[i]; xtk = "b_xt%d" % i; x1 = x1s[i]; x1k = "b_x1%d" % i
                        h2T = h2Ts[i]; h2k = "h2T%d" % i
                        dma("sp", xt[:], x_own[tb * 128:(tb + 1) * 128, :], w=[xtk])
                        pts = []
                        for nb in range(4):
                            pt, pk = kb.ps(); pts.append((pt, pk))
                            for k in range(16):
                                op("pe", lambda e, k=k: e.matmul(pt[:], merged[:, k, tb * 128:(tb + 1) * 128], wo[:, k, nb * 512:(nb + 1) * 512], start=(k == 0), stop=(k == 15)), r=["merged", "wo"], w=[pk], inc=(k == 15))
                            op("act", lambda e, nb=nb, pt=pt: e.activation(out=junk[:, nb * 512:(nb + 1) * 512], in_=pt[:], func=AF.Square, accum_out=ss4[:, nb:nb + 1]), r=[pk], w=["b_junk", "b_ss4"])
                        V(lambda e: e.tensor_reduce(out=rs1[:], in_=ss4[:], axis=AX.X, op=ALU.add), ["b_ss4"], ["b_rs1"])
                        op("act", lambda e: e.activation(out=rs1[:], in_=rs1[:], func=AF.Sqrt, bias=epsc[:], scale=1.0 / D), r=["b_rs1", "epsc"], w=["b_rs1"])
                        V(lambda e: e.reciprocal(out=rs1[:], in_=rs1[:]), ["b_rs1"], ["b_rs1"])
                        for nb in range(4):
                            pt, pk = pts[nb]; cs_ = slice(nb * 512, (nb + 1) * 512)
                            V(lambda e, pt=pt, cs_=cs_: e.tensor_tensor(out=tmp2[:, cs_], in0=pt[:], in1=gbc[:, cs_], op=ALU.mult), [pk, "gbc"], ["b_tmp2"])
                        V(lambda e: e.scalar_tensor_tensor(out=x1[:], in0=tmp2[:], scalar=rs1[:, 0:1], in1=xt[:], op0=ALU.mult, op1=ALU.add), ["b_tmp2", "b_rs1", xtk], [x1k])
                        dma("sp", s_x1[tb * 128:(tb + 1) * 128, :], x1[:], r=[x1k], key="st_x1")

                    def stage_A2(tb):
                        i = tb % 2
                        x1 = x1s[i]; x1k = "b_x1%d" % i
                        h2T = h2Ts[i]; h2k = "h2T%d" % i
                        prenorm_block((x1, x1k, junk2, ss, rstd, tmp, htoks[i], "b_htok%d" % i), None, False, 0, h2T, h2k, names=("b_junk2", "b_ss", "b_rstd", "b_tmp"))
                        dma("sp", s_h2T[:, :, tb * 128:(tb + 1) * 128], h2T[:], r=[h2k], key="st_h2T")

                    def stage_B(tb):
                        i = tb % 2
                        h2T = h2Ts[i]; h2k = "h2T%d" % i
                        pl, plk = kb.ps()
                        for k in range(16):
                            op("pe", lambda e, k=k: e.matmul(pl[:, 0:NE], h2T[:, k, :], wr[:, k, :], start=(k == 0), stop=(k == 15)), r=[h2k, "wr"], w=[plk], inc=(k == 15))
                        op("act", lambda e: e.activation(out=R["sc"][:], in_=pl[:, 0:NE], func=AF.Sigmoid), r=[plk], w=["r_sc"])
                        V(lambda e: e.tensor_tensor(out=R["sel"][:], in0=R["sc"][:], in1=rb[:], op=ALU.add), ["r_sc", "rbias"], ["r_sel"])
                        sel3 = R["sel"][:].rearrange("p (g e) -> p g e", g=8)
                        for g in range(8):
                            V(lambda e, g=g: e.max(out=R["m8g"][:, g, :], in_=sel3[:, g, :]), ["r_sel"], ["r_m8g"])
                        V(lambda e: e.tensor_tensor(out=R["gs"][:], in0=R["m8g"][:, :, 0], in1=R["m8g"][:, :, 1], op=ALU.add), ["r_m8g"], ["r_gs"])
                        V(lambda e: e.max(out=R["gtop"][:], in_=R["gs"][:]), ["r_gs"], ["r_gtop"])
                        V(lambda e: e.tensor_scalar(out=R["gmask"][:], in0=R["gs"][:], scalar1=R["gtop"][:, 3:4], scalar2=None, op0=ALU.is_ge), ["r_gs", "r_gtop"], ["r_gmask"])
                        V(lambda e: e.tensor_scalar(out=R["pen"][:], in0=R["gmask"][:], scalar1=-1.0, scalar2=1e9, op0=ALU.add, op1=ALU.mult), ["r_gmask"], ["r_pen"])
                        selm3 = R["selm"][:].rearrange("p (g e) -> p g e", g=8)
                        for g in range(8):
                            V(lambda e, g=g: e.tensor_scalar(out=selm3[:, g, :], in0=sel3[:, g, :], scalar1=R["pen"][:, g:g + 1], scalar2=None, op0=ALU.add), ["r_sel", "r_pen"], ["r_selm"])
                        V(lambda e: e.max(out=R["top8"][:], in_=R["selm"][:]), ["r_selm"], ["r_top8"])
                        V(lambda e: e.tensor_scalar(out=R["tmask"][:], in0=R["selm"][:], scalar1=R["top8"][:, 5:6], scalar2=None, op0=ALU.is_ge), ["r_selm", "r_top8"], ["r_tmask"])
                        V(lambda e: e.tensor_copy(out=maskall[:, tb, :], in_=R["tmask"][:]), ["r_tmask"], ["maskall"])
                        V(lambda e: e.tensor_tensor(out=R["wraw"][:], in0=R["sc"][:], in1=R["tmask"][:], op=ALU.mult), ["r_sc", "r_tmask"], ["r_wraw"])
                        V(lambda e: e.tensor_reduce(out=R["wsum"][:], in_=R["wraw"][:], axis=AX.X, op=ALU.add), ["r_wraw"], ["r_wsum"])
                        V(lambda e: e.reciprocal(out=R["wsum"][:], in_=R["wsum"][:]), ["r_wsum"], ["r_wsum"])
                        V(lambda e: e.tensor_scalar(out=R["wfull"][:], in0=R["wraw"][:], scalar1=R["wsum"][:, 0:1], scalar2=2.5, op0=ALU.mult, op1=ALU.mult), ["r_wraw", "r_wsum"], ["r_wfull"])
                        pc, pck = kb.ps()
                        for t2 in range(tb + 1):
                            lt = ltri if t2 == tb else ones_b
                            op("pe", lambda e, t2=t2, lt=lt: e.matmul(pc[:, 0:NE], lt[:], maskall[:, t2, :], start=(t2 == 0), stop=(t2 == tb)), r=["ltri", "ones_b", "maskall"], w=[pck], inc=(t2 == tb))
                        V(lambda e: e.tensor_tensor(out=R["ridx"][:], in0=pc[:, 0:NE], in1=ecap[:], op=ALU.add), [pck, "ecap"], ["r_ridx"])
                        V(lambda e: e.tensor_scalar(out=R["okm"][:], in0=pc[:, 0:NE], scalar1=CAP - 0.5, scalar2=None, op0=ALU.is_lt), [pck], ["r_okm"])
                        V(lambda e: e.tensor_scalar(out=R["okt"][:], in0=R["okm"][:], scalar1=-1.0, scalar2=-1.0e6, op0=ALU.add, op1=ALU.mult), ["r_okm"], ["r_okt"])
                        V(lambda e: e.tensor_tensor(out=R["ridx"][:], in0=R["ridx"][:], in1=R["okm"][:], op=ALU.mult), ["r_ridx", "r_okm"], ["r_ridx"])
                        V(lambda e: e.tensor_tensor(out=R["ridx"][:], in0=R["ridx"][:], in1=R["okt"][:], op=ALU.add), ["r_ridx", "r_okt"], ["r_ridx"])
                        for k in range(6):
                            V(lambda e, k=k: e.tensor_scalar(out=R["oh"][:], in0=R["selm"][:], scalar1=R["top8"][:, k:k + 1], scalar2=None, op0=ALU.is_equal), ["r_selm", "r_top8"], ["r_oh"])
                            V(lambda e: e.tensor_tensor(out=R["ot"][:], in0=R["oh"][:], in1=R["ridx"][:], op=ALU.mult), ["r_oh", "r_ridx"], ["r_ot"])
                            V(lambda e, k=k: e.tensor_reduce(out=R["idxf"][:, k:k + 1], in_=R["ot"][:], axis=AX.X, op=ALU.add), ["r_ot"], ["r_idxf"])
                            V(lambda e: e.tensor_tensor(out=R["ot"][:], in0=R["oh"][:], in1=R["wfull"][:], op=ALU.mult), ["r_oh", "r_wfull"], ["r_ot"])
                            V(lambda e, k=k: e.tensor_reduce(out=wk_all[:, tb * 8 + k:tb * 8 + k + 1], in_=R["ot"][:], axis=AX.X, op=ALU.add), ["r_ot"], ["wk_all"])
                        V(lambda e: e.tensor_copy(out=idx_all[:, tb * 8:tb * 8 + 6], in_=R["idxf"][:, 0:6]), ["r_idxf"], ["idx_all"])
                        for k in range(6):
                            kb.idma(s_xe[:, :], bass.IndirectOffsetOnAxis(ap=idx_all[:, tb * 8 + k:tb * 8 + k + 1], axis=0), htoks[i][:, :], None, NE * CAP - 1,
                                    r=["idx_all", "b_htok%d" % i], key="sc_xe%d" % (k % 2))

                    stage_A(0); stage_A(1); stage_A2(0)
                    for tb in range(8):
                        if tb + 2 < 8:
                            stage_A(tb + 2)
                        if tb + 1 < 8:
                            stage_A2(tb + 1)
                        stage_B(tb)
                    if "s_idx" in dbg:
                        dma("sp", s_idx, idx_all[:], r=["idx_all"], key="dbg_idx")
                    kb.barrier()

        if stop_after >= 5:
            with ExitStack() as p5a:
                h2T = sb("e_h2T", [128, 16, NTOK], BF16, p5a)
                dma("sp", h2T[:], s_h2T, w=["e_h2T"])
                wsg = sb("wsg", [128, 16, 512], BF16, p5a); wsu = sb("wsu", [128, 16, 512], BF16, p5a); wsd = sb("wsd", [128, 4, D], BF16, p5a)
                dma("pool", wsg[:], w_sg.rearrange("(k p) n -> p k n", p=128), w=["wsg"]); dma("pool", wsu[:], w_su.rearrange("(k p) n -> p k n", p=128), w=["wsu"])
                dma("pool", wsd[:], w_sd.rearrange("(k p) n -> p k n", p=128), w=["wsd"])
                actS = sb("actS", [128, 4, NTOK], BF16, p5a); sgl = sb("sh_sgl", [128, 512], F32, p5a); ysh = [sb("ysh%d" % i, [128, D], F32, p5a) for i in range(2)]
                for c in range(4):
                    for tt in range(2):
                        sl = slice(tt * 512, (tt + 1) * 512)
                        pg, pgk = kb.ps(); pu, puk = kb.ps()
                        for k in range(16):
                            op("pe", lambda e, k=k: e.matmul(pg[:], wsg[:, k, c * 128:(c + 1) * 128], h2T[:, k, sl], start=(k == 0), stop=(k == 15)), r=["wsg", "e_h2T"], w=[pgk], inc=(k == 15))
                        for k in range(16):
                            op("pe", lambda e, k=k: e.matmul(pu[:], wsu[:, k, c * 128:(c + 1) * 128], h2T[:, k, sl], start=(k == 0), stop=(k == 15)), r=["wsu", "e_h2T"], w=[puk], inc=(k == 15))
                        op("act", lambda e: e.activation(out=sgl[:], in_=pg[:], func=AF.Silu), r=[pgk], w=["sh_sgl"])
                        op("dve", lambda e: e.tensor_tensor(out=actS[:, c, sl], in0=pu[:], in1=sgl[:], op=ALU.mult), r=[puk, "sh_sgl"], w=["actS"])
                for tb in range(8):
                    yt = ysh[tb % 2]; ytk = "ysh%d" % (tb % 2)
                    for nb in range(4):
                        pt, pk = kb.ps()
                        for k in range(4):
                            op("pe", lambda e, k=k: e.matmul(pt[:], actS[:, k, tb * 128:(tb + 1) * 128], wsd[:, k, nb * 512:(nb + 1) * 512], start=(k == 0), stop=(k == 3)), r=["actS", "wsd"], w=[pk], inc=(k == 3))
                        if nb % 2 == 0:
                            op("act", lambda e, pt=pt, nb=nb: e.copy(out=yt[:, nb * 512:(nb + 1) * 512], in_=pt[:]), r=[pk], w=[ytk])
                        else:
                            op("dve", lambda e, pt=pt, nb=nb: e.tensor_copy(out=yt[:, nb * 512:(nb + 1) * 512], in_=pt[:]), r=[pk], w=[ytk])
                    dma("sp", s_sh[tb * 128:(tb + 1) * 128, :], yt[:], r=[ytk], key="st_sh")
                kb.barrier()
            NSB = CAP // 128
            with ExitStack() as p5b:
                wgs_ = [sb("xwg%d" % i, [128, 16, 512], BF16, p5b) for i in range(2)]; wus_ = [sb("xwu%d" % i, [128, 16, 512], BF16, p5b) for i in range(2)]
                wds_ = [sb("xwd%d" % i, [128, 4, D], BF16, p5b) for i in range(2)]
                xtok = [[sb("xtok%d_%d" % (i, j), [128, D], BF16, p5b) for j in range(NSB)] for i in range(2)]
                xeTs = [sb("xeT%d" % i, [128, 16, CAP], BF16, p5b) for i in range(2)]
                actE = sb("actE", [128, 4, CAP], BF16, p5b); sges = [sb("sge%d" % i, [128, CAP], F32, p5b) for i in range(2)]
                yes = [sb("ye%d" % i, [128, D], BF16, p5b) for i in range(3)]
                cnt = {"ny": 0, "ns": 0}

                def issue_loads(ex):
                    i = ex % 2
                    dma("pool", wgs_[i][:], w_eg[ex].rearrange("(k p) n -> p k n", p=128), w=["xwg%d" % i])
                    dma("pool", wus_[i][:], w_eu[ex].rearrange("(k p) n -> p k n", p=128), w=["xwu%d" % i])
                    dma("pool", wds_[i][:], w_ed[ex].rearrange("(k p) n -> p k n", p=128), w=["xwd%d" % i])
                    for sbk in range(NSB):
                        r0 = ex * CAP + sbk * 128
                        dma("sp", xtok[i][sbk][:], s_xe[r0:r0 + 128, :], w=["xtok%d_%d" % (i, sbk)])

                def emit_T(ex):
                    i = ex % 2
                    xeT = xeTs[i]; xk = "xeT%d" % i
                    for sbk in range(NSB):
                        xk_ = xtok[i][sbk]; xkk = "xtok%d_%d" % (i, sbk)
                        for half in range(2):
                            pt, pk = kb.pb()
                            for j in range(8):
                                fc = half * 8 + j
                                op("pe", lambda e, j=j, fc=fc: e.transpose(pt[:, j * 128:(j + 1) * 128], xk_[:, fc * 128:(fc + 1) * 128], ident_b[:]), r=[xkk, "ident_b"], w=[pk], inc=(j == 7))
                            if half == 0:
                                op("act", lambda e: e.copy(out=xeT[:, 0:8, sbk * 128:(sbk + 1) * 128], in_=pt[:].rearrange("p (c t) -> p c t", c=8)), r=[pk], w=[xk])
                            else:
                                op("dve", lambda e: e.tensor_copy(out=xeT[:, 8:16, sbk * 128:(sbk + 1) * 128], in_=pt[:].rearrange("p (c t) -> p c t", c=8)), r=[pk], w=[xk])

                def emit_GU(ex):
                    i = ex % 2
                    xeT = xeTs[i]; xk = "xeT%d" % i
                    for c in range(4):
                        pg, pgk = kb.ps(); pu, puk = kb.ps()
                        for k in range(16):
                            op("pe", lambda e, k=k: e.matmul(pg[:, 0:CAP], wgs_[i][:, k, c * 128:(c + 1) * 128], xeT[:, k, :], start=(k == 0), stop=(k == 15)), r=["xwg%d" % i, xk], w=[pgk], inc=(k == 15))
                        for k in range(16):
                            op("pe", lambda e, k=k: e.matmul(pu[:, 0:CAP], wus_[i][:, k, c * 128:(c + 1) * 128], xeT[:, k, :], start=(k == 0), stop=(k == 15)), r=["xwu%d" % i, xk], w=[puk], inc=(k == 15))
                        sge = sges[cnt["ns"] % 2]; sgk = "sge%d" % (cnt["ns"] % 2); cnt["ns"] += 1
                        op("act", lambda e: e.activation(out=sge[:], in_=pg[:, 0:CAP], func=AF.Silu), r=[pgk], w=[sgk])
                        op("dve", lambda e: e.tensor_tensor(out=actE[:, c, :], in0=pu[:, 0:CAP], in1=sge[:], op=ALU.mult), r=[puk, sgk], w=["actE"])

                def emit_DN(ex):
                    i = ex % 2
                    for sbk in range(NSB):
                        yt = yes[cnt["ny"] % 3]; ytk = "ye%d" % (cnt["ny"] % 3); cnt["ny"] += 1
                        for nb in range(4):
                            pt, pk = kb.ps()
                            for k in range(4):
                                op("pe", lambda e, k=k: e.matmul(pt[:], actE[:, k, sbk * 128:(sbk + 1) * 128], wds_[i][:, k, nb * 512:(nb + 1) * 512], start=(k == 0), stop=(k == 3)), r=["actE", "xwd%d" % i], w=[pk], inc=(k == 3))
                            if nb % 2 == 0:
                                op("act", lambda e, pt=pt, nb=nb: e.copy(out=yt[:, nb * 512:(nb + 1) * 512], in_=pt[:]), r=[pk], w=[ytk])
                            else:
                                op("dve", lambda e, pt=pt, nb=nb: e.tensor_copy(out=yt[:, nb * 512:(nb + 1) * 512], in_=pt[:]), r=[pk], w=[ytk])
                        r0 = ex * CAP + sbk * 128
                        dma("sp", s_ye[r0:r0 + 128, :], yt[:], r=[ytk], key="st_" + ytk)

                issue_loads(0); issue_loads(1)
                emit_T(0)
                for ex in range(NE):
                    emit_GU(ex)
                    if ex + 1 < NE:
                        emit_T(ex + 1)
                    emit_DN(ex)
                    if ex + 2 < NE:
                        issue_loads(ex + 2)
                kb.barrier()
            with ExitStack() as p5c:
                gbc2 = sb("gbc2", [128, D], F32, p5c)
                build_bc(gbc2, "gbc2", 5, p5c)
                accs = [sb("acc%d" % i, [128, D], F32, p5c) for i in range(2)]; gts = [sb("gt%d" % i, [128, D], BF16, p5c) for i in range(12)]
                x1s = [sb("c_x1%d" % i, [128, D], F32, p5c) for i in range(2)]; cj = sb("c_junk", [128, D], BF16, p5c); css = sb("c_ss", [128, 1], F32, p5c)
                ctmp = sb("c_tmp", [128, D], F32, p5c); outs = [sb("c_out%d" % i, [128, D], F32, p5c) for i in range(2)]
                ng = 0
                for tb in range(8):
                    i = tb % 2
                    acc = accs[i]; acck = "acc%d" % i
                    dma("sp", acc[:], s_sh[tb * 128:(tb + 1) * 128, :], w=[acck])
                    dma("sp", x1s[i][:], s_x1[tb * 128:(tb + 1) * 128, :], w=["c_x1%d" % i])
                    for k in range(6):
                        gt = gts[i * 6 + k]; gtk = "gt%d" % (i * 6 + k)
                        kb.idma(gt[:, :], None, s_ye[:, :], bass.IndirectOffsetOnAxis(ap=idx_all[:, tb * 8 + k:tb * 8 + k + 1], axis=0), NE * CAP - 1, r=["idx_all"], w=[gtk], key="ga_" + gtk)
                    for k in range(6):
                        gt = gts[i * 6 + k]; gtk = "gt%d" % (i * 6 + k)
                        op("dve", lambda e, k=k: e.scalar_tensor_tensor(out=acc[:], in0=gt[:], scalar=wk_all[:, tb * 8 + k:tb * 8 + k + 1], in1=acc[:], op0=ALU.mult, op1=ALU.add), r=[gtk, "wk_all", acck], w=[acck])
                    op("act", lambda e: e.activation(out=cj[:], in_=acc[:], func=AF.Square, accum_out=css[:]), r=[acck], w=["c_junk", "c_ss"])
                    op("act", lambda e: e.activation(out=css[:], in_=css[:], func=AF.Sqrt, bias=epsc[:], scale=1.0 / D), r=["c_ss", "epsc"], w=["c_ss"])
                    op("dve", lambda e: e.reciprocal(out=css[:], in_=css[:]), r=["c_ss"], w=["c_ss"])
                    op("dve", lambda e: e.tensor_tensor(out=ctmp[:], in0=acc[:], in1=gbc2[:], op=ALU.mult), r=[acck, "gbc2"], w=["c_tmp"])
                    op("dve", lambda e: e.scalar_tensor_tensor(out=outs[i][:], in0=ctmp[:], scalar=css[:, 0:1], in1=x1s[i][:], op0=ALU.mult, op1=ALU.add), r=["c_tmp", "c_ss", "c_x1%d" % i], w=["c_out%d" % i])
                    dma("sp", out_d[tb * 128:(tb + 1) * 128, :], outs[i][:], r=["c_out%d" % i], key="st_out")
                kb.barrier()

        kb.barrier()
    return nc


def host_inputs(inputs, core):
    b, half = core // 2, core % 2
    f32 = np.float32
    x = np.asarray(inputs["x"], f32)
    pos = np.asarray(inputs["positions"]).astype(np.int32)
    own = slice(half * NTOK, (half + 1) * NTOK)
    m = {}
    m["x_own"] = np.ascontiguousarray(x[b, own])
    if half == 1:
        m["x_ctx"] = np.ascontiguousarray(x[b, 0:NTOK])
        pctx = pos[b, 0:NTOK]
    else:
        m["x_ctx"] = np.ascontiguousarray(x[b, own])
        pctx = np.full((NTOK,), 1 << 22, np.int32)
    pext = np.concatenate([pctx, pos[b, own]]).astype(np.int32)
    m["pos_row"] = pext.reshape(1, NEXT)
    m["pos_col"] = np.ascontiguousarray(pext.reshape(16, 128).T)
    m["c_col"] = np.ascontiguousarray(np.asarray(inputs["c"], f32)[b].reshape(16, 128).T)
    m["flag"] = np.full((128, 1), float(half), f32)
    m["ident"] = np.eye(128, dtype=f32)
    invf = (10000.0 ** (-np.arange(32, dtype=np.float32) / 32.0)).astype(f32)
    m["invf"] = (np.concatenate([invf, invf]) / f32(TWO_PI)).astype(f32).reshape(64, 1)
    m["iota128"] = np.tile(np.arange(128, dtype=f32)[None, :], (128, 1))
    m["b_ada"] = np.asarray(inputs["b_ada"], f32).reshape(1, 6 * D)
    gs = [np.asarray(inputs[k], f32)[0].reshape(16, 128).T for k in ("g_pre_mix", "g_post_mix", "g_pre_ffn", "g_post_ffn")]
    m["gvec"] = np.ascontiguousarray(np.concatenate(gs, axis=1))
    m["gq"] = np.ascontiguousarray(np.asarray(inputs["g_q"], f32)[0].reshape(4, 128).T)
    m["gkv"] = np.ascontiguousarray(np.asarray(inputs["g_kv"], f32)[0].reshape(4, 128).T)
    m["w_ada"] = np.asarray(inputs["w_ada"], f32)[0]
    m["w_in"] = np.asarray(inputs["w_in"], f32)[0]
    for k in ("w_uq", "w_uk", "w_uv", "w_glu", "w_br_mla", "w_br_s5", "w_out", "w_router", "w_exp_gate", "w_exp_up", "w_exp_down", "w_sh_gate", "w_sh_up", "w_sh_down"):
        m[k] = np.asarray(inputs[k], f32)[0]
    m["router_bias"] = np.asarray(inputs["router_bias"], f32).reshape(1, NE)
    m["ecap"] = (np.arange(NE, dtype=f32) * CAP).reshape(1, NE)
    m["ltri"] = np.triu(np.ones((128, 128), f32), 1)

    def state_layout(a):
        a = np.asarray(a, f32)
        r = a.reshape((32, 2) + a.shape[1:])
        r = np.moveaxis(r, 0, 2)
        return np.ascontiguousarray(r.reshape((128, 32) + a.shape[2:]))
    m["a_re_sl"] = state_layout(inputs["a_re"][0]); m["a_im_sl"] = state_layout(inputs["a_im"][0])
    m["ldt2"] = np.ascontiguousarray(np.asarray(inputs["log_dt"], f32)[0].reshape(32, 2).T)
    m["b_re_sl"] = state_layout(inputs["b_re"][0]); m["b_im_sl"] = state_layout(inputs["b_im"][0])
    m["c_re_sl"] = state_layout(np.swapaxes(np.asarray(inputs["c_re"], f32)[0], 1, 2))
    m["c_im_sl"] = state_layout(np.swapaxes(np.asarray(inputs["c_im"], f32)[0], 1, 2))
    m["dcol"] = np.ascontiguousarray(np.asarray(inputs["d_skip"], f32)[0].reshape(8, 128).T)
    return m


def kernel(**inputs):
    nc = build_program()
    in_maps = [host_inputs(inputs, c) for c in range(8)]
    res = run_bass_kernel_spmd(nc, in_maps, core_ids=list(range(8)))
    out = np.zeros((4, 2048, D), np.float32)
    for c in range(8):
        out[c // 2, (c % 2) * NTOK:(c % 2 + 1) * NTOK] = res.results[c]["out"]
    return out
```
